# Optimizing a Trainium2 kernel written in Bass

```python
import jax, jax.numpy as jnp
from jax import lax
import numpy as np

D_MODEL = 2048
BATCH = 8
SEQ = 2048
DEPTH = 1
DEC_BATCH = 32
DEC_SEQ = 4
PAST_LEN = 8192
PAGE_SIZE = 128

N_HEADS = 8
N_KV_HEADS = 2
HEAD_DIM = 128
IDX_HEADS = 16
IDX_DIM = 64
TOPK_MAX = 256
Q_BLOCK = 128
D_CONV = 1024
CONV_WIDTH = 31
N_EXPERTS = 64
N_GROUPS = 8
TOPK_GROUPS = 4
TOP_K = 8
MOE_HIDDEN = 512
SHARED_HIDDEN = 512
ROUTED_SCALE = 2.5
MOE_BLOCK = 128
EPS = 1e-6

D_Q = N_HEADS * HEAD_DIM
D_KV = N_KV_HEADS * HEAD_DIM
D_QI = IDX_HEADS * IDX_DIM
SPLITS = (D_Q, D_KV, D_KV, D_QI, IDX_DIM, IDX_HEADS, 2 * D_CONV, D_MODEL, D_MODEL)
D_IN = sum(SPLITS)
SPLIT_OFFSETS = tuple(int(o) for o in np.cumsum(SPLITS)[:-1])

kernel_name = 'hybrid_dsa_conformer_moe_step'


def _rmsnorm(x, g):
    xf = x.astype(jnp.float32)
    y = xf * lax.rsqrt(jnp.mean(xf * xf, axis=-1, keepdims=True) + EPS)
    return (y * g.astype(jnp.float32)).astype(x.dtype)


def _layernorm(x, g, b):
    xf = x.astype(jnp.float32)
    mu = jnp.mean(xf, axis=-1, keepdims=True)
    var = jnp.mean(jnp.square(xf - mu), axis=-1, keepdims=True)
    y = (xf - mu) * lax.rsqrt(var + EPS) * g.astype(jnp.float32) + b.astype(jnp.float32)
    return y.astype(x.dtype)


def _modulation(c, w_ada, b_ada):
    m = (c @ w_ada + b_ada).reshape(c.shape[0], 1, 6, D_MODEL)
    return [m[:, :, i] for i in range(6)]


def _mix_inputs(x, shift, scale, g_pre, w_in):
    B, T, _ = x.shape
    h = _rmsnorm(x, g_pre) * (1 + scale) + shift
    q, k, v, qi, ki, wi, conv_in, ga, gb = jnp.split(h @ w_in, SPLIT_OFFSETS, axis=-1)
    q = q.reshape(B, T, N_HEADS, HEAD_DIM)
    k = k.reshape(B, T, N_KV_HEADS, HEAD_DIM)
    v = v.reshape(B, T, N_KV_HEADS, HEAD_DIM)
    qi = qi.reshape(B, T, IDX_HEADS, IDX_DIM)
    wi = wi * IDX_HEADS ** -0.5
    a, b = jnp.split(conv_in, 2, axis=-1)
    u = a * jax.nn.sigmoid(b)
    return q, k, v, qi, ki, wi, u, ga, gb


def _index_scores(qi, wi, ki, q_pos, k_pos):
    dots = jnp.einsum('bthd,bsd->bths', qi, ki) * IDX_DIM ** -0.5
    s = jnp.einsum('bth,bths->bts', wi, jax.nn.relu(dots)).astype(jnp.float32)
    return jnp.where(k_pos[None, None, :] <= q_pos[None, :, None], s, -jnp.inf)


def _select(scores, q_pos, topk):
    _, idx = lax.top_k(scores, topk)
    valid = idx <= q_pos[None, :, None]
    return idx, valid


def _sparse_attend(q, k_sel, v_sel, valid):
    B, T = q.shape[:2]
    qg = q.reshape(B, T, N_KV_HEADS, N_HEADS // N_KV_HEADS, HEAD_DIM)
    s = jnp.einsum('btngd,btsnd->btngs', qg, k_sel).astype(jnp.float32) * HEAD_DIM ** -0.5
    s = jnp.where(valid[:, :, None, None, :], s, -jnp.inf)
    p = jax.nn.softmax(s, axis=-1).astype(v_sel.dtype)
    o = jnp.einsum('btngs,btsnd->btngd', p, v_sel)
    return o.reshape(B, T, D_Q)


def _attn_prompt(q, k, v, qi, wi, ki):
    B, S = q.shape[:2]
    topk = min(TOPK_MAX, S // 4)
    n_blk = S // Q_BLOCK
    k_pos = jnp.arange(S)
    gather = jax.vmap(lambda a, i: a[i])

    def to_blocks(a):
        return jnp.moveaxis(a.reshape(B, n_blk, Q_BLOCK, *a.shape[2:]), 1, 0)

    def one_block(args):
        qb, qib, wib, pos = args
        sc = _index_scores(qib, wib, ki, pos, k_pos)
        idx, valid = _select(sc, pos, topk)
        return _sparse_attend(qb, gather(k, idx), gather(v, idx), valid)

    pos_blocks = jnp.arange(S).reshape(n_blk, Q_BLOCK)
    out = lax.map(one_block, (to_blocks(q), to_blocks(qi), to_blocks(wi), pos_blocks))
    return jnp.moveaxis(out, 0, 1).reshape(B, S, D_Q)


def _attn_sample(q, k_new, v_new, qi, wi, ki_new, cache_k, cache_v, cache_kidx, page_table):
    DB, T = q.shape[:2]
    P = page_table.shape[1] * PAGE_SIZE
    L = P + T
    topk = min(TOPK_MAX, L // 4)
    ki_past = cache_kidx[page_table].reshape(DB, P, IDX_DIM)
    ki_all = jnp.concatenate([ki_past, ki_new.astype(ki_past.dtype)], axis=1)
    q_pos = P + jnp.arange(T)
    sc = _index_scores(qi, wi, ki_all, q_pos, jnp.arange(L))
    idx, valid = _select(sc, q_pos, topk)
    in_past = idx < P
    pidx = jnp.minimum(idx, P - 1)
    phys = jnp.take_along_axis(page_table, (pidx // PAGE_SIZE).reshape(DB, -1), axis=1).reshape(idx.shape)
    off = pidx % PAGE_SIZE
    nidx = jnp.clip(idx - P, 0, T - 1)
    gather_new = jax.vmap(lambda a, i: a[i])

    def pick(cache, new):
        past = cache[phys, off]
        cur = gather_new(new, nidx).astype(past.dtype)
        return jnp.where(in_past[..., None, None], past, cur)

    return _sparse_attend(q, pick(cache_k, k_new), pick(cache_v, v_new), valid)


def _conv_branch(u_ext, w_dw, b_dw, ln_g, ln_b, w_pw2):
    y = lax.conv_general_dilated(u_ext, w_dw[:, None, :], window_strides=(1,), padding='VALID',
                                 dimension_numbers=('NWC', 'WIO', 'NWC'),
                                 feature_group_count=D_CONV) + b_dw
    y = _layernorm(y, ln_g, ln_b)
    return jax.nn.silu(y) @ w_pw2


def _mix_output(x, attn_o, conv_o, ga, gb, gate, w_oa, w_o, g_post):
    merged = jax.nn.sigmoid(ga) * (attn_o @ w_oa) + jax.nn.sigmoid(gb) * conv_o
    return x + gate * _rmsnorm(merged @ w_o, g_post)


def _swiglu(x, w_gate, w_up, w_down):
    return (jax.nn.silu(x @ w_gate) * (x @ w_up)) @ w_down


def _route(h, w_router, b_router):
    N = h.shape[0]
    s = jax.nn.sigmoid((h @ w_router).astype(jnp.float32))
    sel = s + b_router.astype(jnp.float32)
    grp = sel.reshape(N, N_GROUPS, N_EXPERTS // N_GROUPS)
    gscore = lax.top_k(grp, 2)[0].sum(-1)
    _, gidx = lax.top_k(gscore, TOPK_GROUPS)
    gmask = jax.nn.one_hot(gidx, N_GROUPS, dtype=jnp.float32).sum(1) > 0
    sel = jnp.where(jnp.repeat(gmask, N_EXPERTS // N_GROUPS, axis=1), sel, -jnp.inf)
    _, eidx = lax.top_k(sel, TOP_K)
    w = jnp.take_along_axis(s, eidx, axis=1)
    w = w / jnp.sum(w, axis=-1, keepdims=True) * ROUTED_SCALE
    return eidx, w


def _moe_routed(h, eidx, ew, w_gate, w_up, w_down):
    N, D = h.shape
    M = N * TOP_K
    flat_e = eidx.reshape(M)
    order = jnp.argsort(flat_e)
    e_sorted = flat_e[order]
    tok = order // TOP_K
    counts = jnp.bincount(flat_e, length=N_EXPERTS)
    padded = (counts + MOE_BLOCK - 1) // MOE_BLOCK * MOE_BLOCK
    pad_end = jnp.cumsum(padded)
    start = jnp.cumsum(counts) - counts
    dest = (pad_end - padded)[e_sorted] + jnp.arange(M) - start[e_sorted]
    n_blocks = -(-M // MOE_BLOCK) + N_EXPERTS
    rows = jnp.zeros((n_blocks * MOE_BLOCK,), jnp.int32).at[dest].set(tok)
    blk_e = jnp.minimum(jnp.searchsorted(pad_end, jnp.arange(n_blocks) * MOE_BLOCK, side='right'),
                        N_EXPERTS - 1)
    xb = h[rows].reshape(n_blocks, MOE_BLOCK, D)

    def expert_block(args):
        xe, e = args
        return _swiglu(xe, w_gate[e], w_up[e], w_down[e])

    yb = lax.map(expert_block, (xb, blk_e)).reshape(n_blocks * MOE_BLOCK, D)
    contrib = yb[dest] * ew.reshape(M)[order][:, None].astype(yb.dtype)
    return jax.ops.segment_sum(contrib, tok, num_segments=N)


def _ffn_sublayer(x, shift, scale, gate, g_pre, g_post, w_router, b_router,
                  w_exp_gate, w_exp_up, w_exp_down, w_sh_gate, w_sh_up, w_sh_down):
    B, T, D = x.shape
    h = (_rmsnorm(x, g_pre) * (1 + scale) + shift).reshape(B * T, D)
    eidx, ew = _route(h, w_router, b_router)
    y = _swiglu(h, w_sh_gate, w_sh_up, w_sh_down) + _moe_routed(h, eidx, ew, w_exp_gate, w_exp_up, w_exp_down)
    return x + gate * _rmsnorm(y.reshape(B, T, D), g_post)


def setup_inputs(seed: int = 0) -> dict:
    key = jax.random.key(seed)
    ks = iter(jax.random.split(key, 48))

    def nrm(shape, scale=1.0):
        return jax.random.normal(next(ks), shape, jnp.float32) * scale

    n_pages = PAST_LEN // PAGE_SIZE
    used = DEC_BATCH * n_pages
    n_pool = used + max(1, used // 4)
    L = DEPTH
    page_table = jax.random.permutation(next(ks), n_pool)[:used].reshape(DEC_BATCH, n_pages).astype(jnp.int32)
    return {
        'x_prompt': nrm((BATCH, SEQ, D_MODEL)),
        'x_sample': nrm((DEC_BATCH, DEC_SEQ, D_MODEL)),
        'cache_k': nrm((L, n_pool, PAGE_SIZE, N_KV_HEADS, HEAD_DIM)),
        'cache_v': nrm((L, n_pool, PAGE_SIZE, N_KV_HEADS, HEAD_DIM)),
        'cache_kidx': nrm((L, n_pool, PAGE_SIZE, IDX_DIM)),
        'state_conv': nrm((L, DEC_BATCH, CONV_WIDTH - 1, D_CONV), 0.5),
        'page_table': page_table,
        'c_prompt': nrm((BATCH, D_MODEL)),
        'c_sample': nrm((DEC_BATCH, D_MODEL)),
        'w_ada': nrm((L, D_MODEL, 6 * D_MODEL), 0.2 * D_MODEL ** -0.5),
        'b_ada': nrm((L, 6 * D_MODEL), 0.01),
        'g_mix_pre': 1.0 + nrm((L, D_MODEL), 0.01),
        'g_mix_post': 1.0 + nrm((L, D_MODEL), 0.01),
        'w_in': nrm((L, D_MODEL, D_IN), D_MODEL ** -0.5),
        'w_oa': nrm((L, D_Q, D_MODEL), D_Q ** -0.5),
        'w_dw': nrm((L, CONV_WIDTH, D_CONV), CONV_WIDTH ** -0.5),
        'b_dw': nrm((L, D_CONV), 0.01),
        'ln_conv_g': 1.0 + nrm((L, D_CONV), 0.01),
        'ln_conv_b': nrm((L, D_CONV), 0.01),
        'w_pw2': nrm((L, D_CONV, D_MODEL), D_CONV ** -0.5),
        'w_o': nrm((L, D_MODEL, D_MODEL), D_MODEL ** -0.5),
        'g_ffn_pre': 1.0 + nrm((L, D_MODEL), 0.01),
        'g_ffn_post': 1.0 + nrm((L, D_MODEL), 0.01),
        'w_router': nrm((L, D_MODEL, N_EXPERTS), D_MODEL ** -0.5),
        'b_router': nrm((L, N_EXPERTS), 0.01),
        'w_exp_gate': nrm((L, N_EXPERTS, D_MODEL, MOE_HIDDEN), D_MODEL ** -0.5),
        'w_exp_up': nrm((L, N_EXPERTS, D_MODEL, MOE_HIDDEN), D_MODEL ** -0.5),
        'w_exp_down': nrm((L, N_EXPERTS, MOE_HIDDEN, D_MODEL), MOE_HIDDEN ** -0.5),
        'w_sh_gate': nrm((L, D_MODEL, SHARED_HIDDEN), D_MODEL ** -0.5),
        'w_sh_up': nrm((L, D_MODEL, SHARED_HIDDEN), D_MODEL ** -0.5),
        'w_sh_down': nrm((L, SHARED_HIDDEN, D_MODEL), SHARED_HIDDEN ** -0.5),
    }


def reference(x_prompt, x_sample, cache_k, cache_v, cache_kidx, state_conv, page_table, c_prompt, c_sample,
              w_ada, b_ada, g_mix_pre, g_mix_post, w_in, w_oa, w_dw, b_dw, ln_conv_g, ln_conv_b, w_pw2, w_o,
              g_ffn_pre, g_ffn_post, w_router, b_router, w_exp_gate, w_exp_up, w_exp_down,
              w_sh_gate, w_sh_up, w_sh_down):
    xp, xs = x_prompt, x_sample
    kp_l, vp_l, ip_l, cp_l = [], [], [], []
    ks_l, vs_l, is_l, cs_l = [], [], [], []
    for l in range(DEPTH):
        mp = _modulation(c_prompt, w_ada[l], b_ada[l])
        ms = _modulation(c_sample, w_ada[l], b_ada[l])

        q, k, v, qi, ki, wi, u, ga, gb = _mix_inputs(xp, mp[0], mp[1], g_mix_pre[l], w_in[l])
        ao = _attn_prompt(q, k, v, qi, wi, ki)
        u_ext = jnp.pad(u, ((0, 0), (CONV_WIDTH - 1, 0), (0, 0)))
        co = _conv_branch(u_ext, w_dw[l], b_dw[l], ln_conv_g[l], ln_conv_b[l], w_pw2[l])
        xp = _mix_output(xp, ao, co, ga, gb, mp[2], w_oa[l], w_o[l], g_mix_post[l])
        xp = _ffn_sublayer(xp, mp[3], mp[4], mp[5], g_ffn_pre[l], g_ffn_post[l], w_router[l], b_router[l],
                           w_exp_gate[l], w_exp_up[l], w_exp_down[l], w_sh_gate[l], w_sh_up[l], w_sh_down[l])
        kp_l.append(k)
        vp_l.append(v)
        ip_l.append(ki)
        cp_l.append(u[:, -(CONV_WIDTH - 1):])

        q, k, v, qi, ki, wi, u, ga, gb = _mix_inputs(xs, ms[0], ms[1], g_mix_pre[l], w_in[l])
        ao = _attn_sample(q, k, v, qi, wi, ki, cache_k[l], cache_v[l], cache_kidx[l], page_table)
        u_ext = jnp.concatenate([state_conv[l].astype(u.dtype), u], axis=1)
        co = _conv_branch(u_ext, w_dw[l], b_dw[l], ln_conv_g[l], ln_conv_b[l], w_pw2[l])
        xs = _mix_output(xs, ao, co, ga, gb, ms[2], w_oa[l], w_o[l], g_mix_post[l])
        xs = _ffn_sublayer(xs, ms[3], ms[4], ms[5], g_ffn_pre[l], g_ffn_post[l], w_router[l], b_router[l],
                           w_exp_gate[l], w_exp_up[l], w_exp_down[l], w_sh_gate[l], w_sh_up[l], w_sh_down[l])
        ks_l.append(k)
        vs_l.append(v)
        is_l.append(ki)
        cs_l.append(u_ext[:, -(CONV_WIDTH - 1):])

    k_prompt = jnp.stack(kp_l)
    v_prompt = jnp.stack(vp_l)
    kidx_prompt = jnp.stack(ip_l)
    conv_prompt = jnp.stack(cp_l)
    k_sample = jnp.stack(ks_l)
    v_sample = jnp.stack(vs_l)
    kidx_sample = jnp.stack(is_l)
    conv_sample = jnp.stack(cs_l)
    return (xp, xs, k_prompt, v_prompt, kidx_prompt, conv_prompt, k_sample, v_sample, kidx_sample, conv_sample)
```

```python
import contextlib
import numpy as np
import ml_dtypes
import concourse.bass as bass
import concourse.mybir as mybir
from concourse.bass_utils import run_bass_kernel_spmd

F32 = mybir.dt.float32
BF16 = mybir.dt.bfloat16
I32 = mybir.dt.int32
AF = mybir.ActivationFunctionType
ALU = mybir.AluOpType
AX = mybir.AxisListType

T = 2048
NS = 64
NT = T + NS
D = 2048
KC = 16
EPS = 1e-6
CAP = 512
NEXP = 64
NEG = -1.0e30
MNEG = -30000.0
NKS = 8320
BIS_ITERS = 17
STAGES = 99
DEBUG_NAMES = set()

TT = [(0, 512), (512, 512), (1024, 512), (1536, 512), (2048, 64)]
RT = [(i * 128, 128) for i in range(16)] + [(2048, 64)]


class View:
    def __init__(self, buf, ap):
        self.buf = buf
        self.ap = ap

    def __getitem__(self, idx):
        return View(self.buf, self.ap[idx])

    def rearrange(self, pat, **kw):
        return View(self.buf, self.ap.rearrange(pat, **kw))

    def bitcast(self, dt):
        return View(self.buf, self.ap.bitcast(dt))

    def bcast(self, n):
        return View(self.buf, self.ap.partition_broadcast(n))

    def to_broadcast(self, shape):
        return View(self.buf, self.ap.to_broadcast(shape))


class Buf:
    def __init__(self, name, t, space):
        self.name = name
        self.t = t
        self.space = space
        self.writes = {}
        self.reads = {}
        self.dsem = None

    def __getitem__(self, idx):
        return View(self, self.t[idx])

    @property
    def v(self):
        return View(self, self.t if self.space == "dram" else self.t[:])


class SemObj:
    def __init__(self, h):
        self.h = h
        self.cnt = 0


WRITE_KEYS = ("out", "accum_out", "out_max", "out_indices")


class Sched:
    def __init__(self, nc, ndsem=88):
        self.nc = nc
        self.eng = {"pe": nc.tensor, "act": nc.scalar, "dve": nc.vector, "pool": nc.gpsimd, "sp": nc.sync}
        self.esem = {k: SemObj(nc.alloc_semaphore("es_" + k)) for k in ("pe", "act", "dve", "pool")}
        self.esem_ids = {id(v) for v in self.esem.values()}
        self.arrive = nc.alloc_semaphore("bar_arrive")
        self.go = nc.alloc_semaphore("bar_go")
        self.epoch = 0
        self.free_dsems = [SemObj(nc.alloc_semaphore("ds%d" % i)) for i in range(ndsem)]
        self.all_dsems = list(self.free_dsems)
        self.seen = {k: {} for k in self.eng}
        self.scopes = []
        self.pe_pending = False
        self.bregs = {}
        self.uid = 0

    @contextlib.contextmanager
    def scope(self):
        st = contextlib.ExitStack()
        self.scopes.append((st, []))
        try:
            yield
        finally:
            self.barrier()
            st_, bufs = self.scopes.pop()
            for b in bufs:
                if b.dsem is not None:
                    self.free_dsems.append(b.dsem)
                    b.dsem = None
            st_.close()

    def sb(self, name, shape, dt):
        self.uid += 1
        nm = "%s_%d" % (name, self.uid)
        if self.scopes:
            t = self.scopes[-1][0].enter_context(self.nc.sbuf_tensor(nm, list(shape), dt))
        else:
            t = self.nc.alloc_sbuf_tensor(nm, list(shape), dt)
        b = Buf(nm, t, "sb")
        if self.scopes:
            self.scopes[-1][1].append(b)
        return b

    def ps(self, name, shape, dt=F32):
        t = self.nc.alloc_psum_tensor(name, list(shape), dt)
        return Buf(name, t, "ps")

    def dram(self, name, shape, dt, kind="Internal"):
        if kind == "Internal" and name in DEBUG_NAMES:
            kind = "ExternalOutput"
        t = self.nc.dram_tensor(name, list(shape), dt, kind=kind).ap()
        return Buf(name, t, "dram")

    def _dsem(self, b):
        if b.dsem is None:
            b.dsem = self.free_dsems.pop()
        return b.dsem

    def _wait(self, e, deps):
        best = {}
        for (so, val, ep) in deps:
            if ep != self.epoch:
                continue
            k = id(so)
            if k not in best or best[k][1] < val:
                best[k] = (so, val)
        for so, val in best.values():
            if id(so) not in self.esem_ids:
                val = max(val, so.cnt)
            elif e == "pe" and so is self.esem["pe"]:
                continue
            if self.seen[e].get(id(so), 0) >= val:
                continue
            self.eng[e].wait_ge(so.h, val)
            self.seen[e][id(so)] = val

    def _collect(self, kw):
        reads, writes = [], []
        for k, v in kw.items():
            if isinstance(v, View):
                (writes if k in WRITE_KEYS else reads).append(v.buf)
        return reads, writes

    def _record(self, tag, reads, writes, disjoint):
        k = id(tag[0])
        for w in writes:
            if not disjoint:
                w.writes = {}
                w.reads = {}
            if k not in w.writes or w.writes[k][2] != tag[2] or w.writes[k][1] < tag[1]:
                w.writes[k] = tag
        for r in reads:
            if k not in r.reads or r.reads[k][2] != tag[2] or r.reads[k][1] < tag[1]:
                r.reads[k] = tag

    def _deps(self, reads, writes, disjoint):
        deps = []
        for r in reads:
            deps += list(r.writes.values())
        for w in writes:
            deps += list(w.reads.values())
            if not disjoint:
                deps += list(w.writes.values())
        return deps

    def I(self, e, name, disjoint=False, inc=True, extra_reads=(), extra_writes=(), **kw):
        reads, writes = self._collect(kw)
        reads += list(extra_reads)
        writes += list(extra_writes)
        self._wait(e, self._deps(reads, writes, disjoint))
        args = {k: (v.ap if isinstance(v, View) else v) for k, v in kw.items()}
        ins = getattr(self.eng[e], name)(**args)
        so = self.esem[e]
        if inc:
            so.cnt += 1
            ins.then_inc(so.h, 1)
            tag = (so, so.cnt, self.epoch)
        else:
            assert e == "pe"
            tag = (so, so.cnt + 1, self.epoch)
        self._record(tag, reads, writes, disjoint)
        return ins

    def D(self, q, disjoint=False, semof=None, indirect=False, **kw):
        reads, writes = self._collect(kw)
        for k in ("out_offset", "in_offset"):
            if kw.get(k) is not None:
                reads.append(kw[k][0].buf)
        self._wait(q, self._deps(reads, writes, disjoint))
        if semof is None:
            cands = [b for b in writes + reads if b.space == "sb"]
            semof = cands[0] if cands else (writes + reads)[0]
        so = self._dsem(semof)
        args = {}
        for k, v in kw.items():
            if k in ("out_offset", "in_offset"):
                args[k] = None if v is None else bass.IndirectOffsetOnAxis(ap=v[0].ap, axis=v[1])
            else:
                args[k] = v.ap if isinstance(v, View) else v
        if indirect:
            bc = args.get("bounds_check")
            if isinstance(bc, int):
                if bc not in self.bregs:
                    r = self.nc.gpsimd.alloc_register("bnd%d" % bc)
                    self.nc.gpsimd.reg_mov(r, bc)
                    self.bregs[bc] = r
                args["bounds_check"] = self.bregs[bc]
            ins = self.eng[q].indirect_dma_start(**args)
        else:
            ins = self.eng[q].dma_start(**args)
        so.cnt += 16
        ins.then_inc(so.h, 16)
        self._record((so, so.cnt, self.epoch), reads, writes, disjoint)
        return ins

    def barrier(self):
        allsems = list(self.esem.values()) + [s for s in self.all_dsems if s.cnt > 0]
        for e in self.eng:
            for so in allsems:
                if self.seen[e].get(id(so), 0) < so.cnt:
                    self.eng[e].wait_ge(so.h, so.cnt)
                    self.seen[e][id(so)] = so.cnt

    def mm(self, out, pairs, last_inc=True):
        n = len(pairs)
        for i, (l, r) in enumerate(pairs):
            self.I("pe", "matmul", out=out, lhsT=l, rhs=r, start=(i == 0), stop=(i == n - 1),
                   inc=(i == n - 1))


def build(stages=STAGES):
    nc = bass.Bass("TRN2", target_bir_lowering=False)
    S = Sched(nc)
    I_, Dm = S.I, S.D

    def din(name, shape, dt=F32):
        return S.dram(name, shape, dt, "ExternalInput")

    def dout(name, shape, dt=F32):
        return S.dram(name, shape, dt, "ExternalOutput")

    xin = din("xin", [NT, D])
    cT = din("cT", [128, KC, 5])
    w_ada = din("w_ada", [D, 6 * D])
    b_ada = din("b_ada", [1, 6 * D])
    g_mix_pre = din("g_mix_pre", [1, D])
    g_mix_post = din("g_mix_post", [1, D])
    w_in = din("w_in", [D, 8784])
    w_oa = din("w_oa", [1024, D])
    wdwT = din("wdwT", [128, 8, 31])
    bdwT = din("bdwT", [128, 8])
    lngT = din("lngT", [128, 8])
    lnbT = din("lnbT", [128, 8])
    w_pw2 = din("w_pw2", [1024, D])
    w_o = din("w_o", [D, D])
    g_ffn_pre = din("g_ffn_pre", [1, D])
    g_ffn_post = din("g_ffn_post", [1, D])
    w_router = din("w_router", [D, NEXP])
    b_router = din("b_router", [1, NEXP])
    w_eg = din("w_eg", [NEXP + 1, D, 512])
    w_eu = din("w_eu", [NEXP + 1, D, 512])
    w_ed = din("w_ed", [NEXP + 1, 512, D])
    cache_k = din("cache_k", [2560, 128 * 256])
    cache_v = din("cache_v", [2560, 128 * 256])
    cache_ki = din("cache_ki", [2560, 128 * 64])
    state_conv = din("state_conv", [4, 30, 1024])
    ptT = din("ptT", [64, 4], I32)
    c_identb = din("c_identb", [128, 128], BF16)
    c_identf = din("c_identf", [128, 128])
    c_cbP = din("c_cbP", [128, 128])
    c_cbS = din("c_cbS", [16, 128])
    c_triu = din("c_triu", [128, 128], BF16)
    c_eoff = din("c_eoff", [128, NEXP])
    c_vmask = din("c_vmask", [64, 1])
    c_selB = din("c_selB", [4, 16])

    y_out = dout("y_out", [NT, D])
    k_out = dout("k_out", [NT, 256])
    v_out = dout("v_out", [NT, 256])
    ki_out = dout("ki_out", [NT, 64])
    convp_out = dout("convp_out", [30, 1024])
    convs_out = dout("convs_out", [4, 30, 1024])

    mod_d = S.dram("mod_d", [5, 6 * D], F32)
    hT_d = S.dram("hT_d", [128, KC, NT], BF16)
    qT_d = S.dram("qT_d", [128, 8, NT], BF16)
    qiT_d = S.dram("qiT_d", [128, 8, NT], BF16)
    wi_d = S.dram("wi_d", [NT, 16], F32)
    sT_d = S.dram("sT_d", [128, 8, NT], BF16)
    aoT_d = S.dram("aoT_d", [128, 8, NT], BF16)
    mT_d = S.dram("mT_d", [128, KC, NT], BF16)
    x1_d = S.dram("x1_d", [NT, D], F32)
    h2T_d = S.dram("h2T_d", [128, KC, NT], BF16)
    Xs_d = S.dram("Xs_d", [NEXP * CAP, D], BF16)
    Ys_d = S.dram("Ys_d", [NEXP * CAP, D], F32)
    Ysh_d = S.dram("Ysh_d", [NT, D], F32)
    Ks_d = S.dram("Ks_d", [4, NKS, 256], F32)
    Vs_d = S.dram("Vs_d", [4, NKS, 256], F32)
    kis_d = S.dram("kis_d", [4, NKS, 64], F32)

    PS = [S.ps("ps%d" % i, [128, 512], F32) for i in range(6)]
    PB = [S.ps("pb%d" % i, [128, 1024], BF16) for i in range(2)]
    psi = [0]

    def nps():
        psi[0] = (psi[0] + 1) % nrot[0]
        return PS[psi[0]]

    nrot = [4]

    PSO = PS[5]
    PSI = PS[4]

    pbi = [0]

    def npb():
        pbi[0] = (pbi[0] + 1) % len(PB)
        return PB[pbi[0]]

    identb = S.sb("identb", [128, 128], BF16)
    identf = S.sb("identf", [128, 128], F32)
    onesf = S.sb("onesf", [128, 128], F32)
    onesb = S.sb("onesb", [128, 128], BF16)
    Dm("sp", out=identb.v, in_=c_identb.v)
    Dm("sp", out=identf.v, in_=c_identf.v)
    I_("dve", "memset", ap=onesf.v.ap, extra_writes=[onesf], constant=1.0)
    I_("dve", "memset", ap=onesb.v.ap, extra_writes=[onesb], constant=1.0)
    idx8 = S.sb("idx8", [128, 17, 8], I32)
    w8 = S.sb("w8", [128, 17, 8], F32)

    def wview(w, c0, ncol, k0=0, kc=KC):
        return w.v[k0 * 128:(k0 + kc) * 128, c0:c0 + ncol].rearrange("(kc p) n -> p kc n", p=128)

    def load_bc(dst, src_row_view, nparts, p0=0):
        Dm("sp", out=dst[p0:p0 + nparts, :], in_=src_row_view.bcast(nparts), disjoint=(p0 != 0))

    def load_mod(dst, which, sample):
        c0 = which * D
        if not sample:
            load_bc(dst, mod_d.v[0:1, c0:c0 + D], 128)
        else:
            for b in range(4):
                load_bc(dst, mod_d.v[1 + b:2 + b, c0:c0 + D], 16, p0=16 * b)

    def rstd_from_ss(rstd, ss, nr, n):
        I_("dve", "tensor_scalar", out=rstd[:nr, :], in0=ss[:nr, :], scalar1=1.0 / n, scalar2=EPS,
           op0=ALU.mult, op1=ALU.add)
        I_("act", "sqrt", out=rstd[:nr, :], in_=rstd[:nr, :])
        I_("dve", "reciprocal", out=rstd[:nr, :], in_=rstd[:nr, :])

    def transpose_to(dst_fn, src, nr, nchunks, evac="act"):
        for c0 in range(0, nchunks, 4):
            pb = npb()
            for c in range(c0, min(c0 + 4, nchunks)):
                I_("pe", "transpose", out=pb[:, (c - c0) * 128:(c - c0) * 128 + nr],
                   in_=src[:nr, c * 128:(c + 1) * 128], identity=identb[:nr, :nr], disjoint=True)
            for c in range(c0, min(c0 + 4, nchunks)):
                srcv = pb[:, (c - c0) * 128:(c - c0) * 128 + nr]
                if evac == "act":
                    I_("act", "copy", out=dst_fn(c), in_=srcv, disjoint=True)
                else:
                    I_("dve", "tensor_copy", out=dst_fn(c), in_=srcv, disjoint=True)

    with S.scope():
        cTs = S.sb("cTs", [128, KC, 5], F32)
        Dm("sp", out=cTs.v, in_=cT.v)
        wb = [S.sb("wada%d" % i, [128, KC, 512], F32) for i in range(2)]
        bt = [S.sb("bt%d" % i, [5, 512], F32) for i in range(2)]
        mt = [S.sb("mt%d" % i, [5, 512], F32) for i in range(2)]
        for g in range(24):
            w = wb[g % 2]
            Dm("sp", out=w.v, in_=wview(w_ada, g * 512, 512))
            Dm("sp", out=bt[g % 2].v, in_=b_ada.v[0:1, g * 512:(g + 1) * 512].bcast(5))
            ps = nps()
            S.mm(ps[0:5, :], [(cTs[:, kc, :], w[:, kc, :]) for kc in range(KC)])
            I_("dve", "tensor_tensor", out=mt[g % 2].v, in0=ps[0:5, :], in1=bt[g % 2].v, op=ALU.add)
            Dm("sp", out=mod_d.v[:, g * 512:(g + 1) * 512], in_=mt[g % 2].v, disjoint=True)
    if stages <= 1:
        return finish(nc, S, [mod_d])

    with S.scope():
        hT = S.sb("hT", [128, KC, NT], BF16)
        with S.scope():
            gpre = S.sb("gpre", [128, D], F32)
            load_bc(gpre, g_mix_pre.v[0:1, :], 128)
            gs1 = [S.sb("gs1_%d" % i, [128, D], F32) for i in range(2)]
            sh1 = [S.sb("sh1_%d" % i, [128, D], F32) for i in range(2)]
            for s in range(2):
                load_mod(gs1[s], 1, s == 1)
                load_mod(sh1[s], 0, s == 1)
                nr = 128 if s == 0 else 64
                I_("dve", "scalar_tensor_tensor", out=gs1[s][:nr, :], in0=gs1[s][:nr, :], scalar=1.0,
                   in1=gpre[:nr, :], op0=ALU.add, op1=ALU.mult)
            xt = [S.sb("xt%d" % i, [128, D], F32) for i in range(2)]
            junk = S.sb("junk", [128, D], BF16)
            xh = S.sb("xh", [128, D], F32)
            xhb = [S.sb("xhb%d" % i, [128, D], BF16) for i in range(2)]
            ss = [S.sb("ss%d" % i, [128, 1], F32) for i in range(2)]
            rs = [S.sb("rs%d" % i, [128, 1], F32) for i in range(2)]
            for ti, (r0, nr) in enumerate(RT):
                s = 1 if r0 >= T else 0
                x_ = xt[ti % 2]
                Dm("sp", out=x_[:nr, :], in_=xin.v[r0:r0 + nr, :])
                I_("act", "activation", out=junk[:nr, :], in_=x_[:nr, :], func=AF.Square,
                   accum_out=ss[ti % 2][:nr, :])
                rstd_from_ss(rs[ti % 2], ss[ti % 2], nr, D)
                I_("dve", "scalar_tensor_tensor", out=xh[:nr, :], in0=x_[:nr, :], scalar=rs[ti % 2][:nr, 0:1],
                   in1=gs1[s][:nr, :], op0=ALU.mult, op1=ALU.mult)
                hb = xhb[ti % 2]
                I_("dve", "tensor_tensor", out=hb[:nr, :], in0=xh[:nr, :], in1=sh1[s][:nr, :], op=ALU.add)
                transpose_to(lambda c: hT[:, c, r0:r0 + nr], hb, nr, KC)
            Dm("sp", out=hT_d.v, in_=hT.v)

        with S.scope():
            wbuf = [S.sb("wbuf%d" % i, [128, KC, 512], BF16) for i in range(2)]
            qo = [S.sb("qo%d" % i, [128, 512], BF16) for i in range(2)]
            gthunks = gather_prep(nc, S, locals())
            gi = 0
            for (dst, col0) in ((qT_d, 0), (qiT_d, 1536)):
                for g in range(2):
                    w = wbuf[gi % 2]
                    gi += 1
                    Dm("pool", out=w.v, in_=wview(w_in, col0 + g * 512, 512))
                    if gi >= 2:
                        for _ in range(12):
                            if gthunks:
                                gthunks.pop(0)()
                    for (t0, nt) in TT:
                        for hh in range(4):
                            ps = nps()
                            S.mm(ps[:, :nt], [(w[:, kc, hh * 128:(hh + 1) * 128], hT[:, kc, t0:t0 + nt])
                                              for kc in range(KC)])
                            q_ = qo[(hh) % 2]
                            I_("act", "copy", out=q_[:, :nt], in_=ps[:, :nt])
                            Dm("sp", out=dst.v[:, g * 4 + hh, t0:t0 + nt], in_=q_[:, :nt], disjoint=True)
            w = wbuf[gi % 2]
            gi += 1
            Dm("pool", out=w.v, in_=wview(w_in, 1024, 512))
            while gthunks:
                gthunks.pop(0)()
            wsm = S.sb("wsm", [128, KC, 80], BF16)
            Dm("pool", out=wsm.v, in_=wview(w_in, 2560, 80))
            kvt = [S.sb("kvt%d" % i, [128, 512], F32) for i in range(2)]
            kwt = [S.sb("kwt%d" % i, [128, 80], F32) for i in range(2)]
            wit = [S.sb("wit%d" % i, [128, 16], F32) for i in range(2)]
            for ti, (r0, nr) in enumerate(RT):
                ps = nps()
                S.mm(ps[:nr, :], [(hT[:, kc, r0:r0 + nr], w[:, kc, :]) for kc in range(KC)])
                kv = kvt[ti % 2]
                I_("act", "copy", out=kv[:nr, :], in_=ps[:nr, :])
                Dm("sp", out=k_out.v[r0:r0 + nr, :], in_=kv[:nr, 0:256], disjoint=True)
                Dm("sp", out=v_out.v[r0:r0 + nr, :], in_=kv[:nr, 256:512], disjoint=True)
                ps = nps()
                S.mm(ps[:nr, :80], [(hT[:, kc, r0:r0 + nr], wsm[:, kc, :]) for kc in range(KC)])
                kw_ = kwt[ti % 2]
                I_("dve", "tensor_copy", out=kw_[:nr, :], in_=ps[:nr, :80])
                Dm("sp", out=ki_out.v[r0:r0 + nr, :], in_=kw_[:nr, 0:64], disjoint=True)
                I_("dve", "tensor_scalar", out=wit[ti % 2][:nr, :], in0=kw_[:nr, 64:80], scalar1=1.0 / 32.0,
                   scalar2=None, op0=ALU.mult)
                Dm("sp", out=wi_d.v[r0:r0 + nr, :], in_=wit[ti % 2][:nr, :], disjoint=True)
        if stages <= 2:
            S.barrier()
            return finish(nc, S, [k_out, v_out, ki_out, wi_d, qT_d, qiT_d])

        with S.scope():
            conv_stage(nc, S, locals())
    if stages <= 3:
        return finish(nc, S, [k_out, v_out, ki_out, convp_out, convs_out, sT_d])
    gather_stage(nc, S, locals())
    attn_stage(nc, S, locals())
    if stages <= 4:
        return finish(nc, S, [k_out, v_out, ki_out, convp_out, convs_out, aoT_d])
    merge_stage(nc, S, locals())
    if stages <= 5:
        return finish(nc, S, [k_out, v_out, ki_out, convp_out, convs_out, x1_d, h2T_d, Xs_d])
    expert_stage(nc, S, locals())
    combine_stage(nc, S, locals())
    return finish(nc, S, [y_out, k_out, v_out, ki_out, convp_out, convs_out])


def conv_stage(nc, S, L):
    I_, Dm = S.I, S.D
    hT, w_in, nps, npb = L["hT"], L["w_in"], L["nps"], L["npb"]
    identf, onesf, wview = L["identf"], L["onesf"], L["wview"]
    wdwT, bdwT, lngT, lnbT = L["wdwT"], L["bdwT"], L["lngT"], L["lnbT"]
    state_conv, convp_out, convs_out, sT_d = L["state_conv"], L["convp_out"], L["convs_out"], L["sT_d"]
    wdw = S.sb("wdw", [128, 8, 31], F32)
    bdw = S.sb("bdw", [128, 8], F32)
    lng = S.sb("lng", [128, 8], F32)
    lnb = S.sb("lnb", [128, 8], F32)
    for d_, s_ in ((wdw, wdwT), (bdw, bdwT), (lng, lngT), (lnb, lnbT)):
        Dm("sp", out=d_.v, in_=s_.v)
    stcs = [S.sb("stc%d" % i, [30, 4, 128], F32) for i in range(2)]
    Dm("sp", out=convs_out.v[:, 0:26, :], in_=state_conv.v[:, 4:30, :], disjoint=True, semof=convs_out)
    wa = [S.sb("wa%d" % i, [128, KC, 128], BF16) for i in range(2)]
    wb = [S.sb("wb%d" % i, [128, KC, 128], BF16) for i in range(2)]
    ue = [S.sb("ue%d" % i, [128, 30 + T], F32) for i in range(1)]
    ues = [S.sb("ues%d" % i, [128, 4, 34], F32) for i in range(2)]
    sg = [S.sb("sg%d" % i, [128, 512], F32) for i in range(2)]
    us = S.sb("us", [128, 64], F32)
    acc = [S.sb("acc%d" % i, [128, NT], F32) for i in range(2)]
    accs = [S.sb("accs%d" % i, [128, 4, 4], F32) for i in range(2)]
    ybf = S.sb("ybf", [128, 8, NT], BF16)
    ub = S.sb("ub", [128, 30 + T], BF16)
    dg = [S.sb("dg%d" % i, [128, 31, 128], BF16) for i in range(2)]
    st1 = S.sb("st1", [128, NT], F32)
    st2 = S.sb("st2", [128, NT], F32)
    sq = [S.sb("sq%d" % i, [128, 512], F32) for i in range(2)]
    cpo = [S.sb("cpo%d" % i, [30, 128], F32) for i in range(2)]
    cso = [S.sb("cso%d" % i, [4, 128], F32) for i in range(4)]
    I_("dve", "memset", ap=ybf.v.ap, extra_writes=[ybf], constant=0.0)
    for i in range(1):
        I_("dve", "memset", ap=ue[i][:, 0:30].ap, extra_writes=[ue[i]], constant=0.0)
    for cc in range(8):
        a_, b_ = wa[cc % 2], wb[cc % 2]
        Dm("pool", out=a_.v, in_=wview(w_in, 2640 + cc * 128, 128))
        Dm("pool", out=b_.v, in_=wview(w_in, 3664 + cc * 128, 128))
        u_, us_ = ue[0], ues[cc % 2]
        stc = stcs[cc % 2]
        Dm("sp", out=stc.v, in_=state_conv.v[:, :, cc * 128:(cc + 1) * 128].rearrange("b t c -> t b c"))
        for b in range(4):
            ps = nps()
            I_("pe", "transpose", out=ps[:, 0:30], in_=stc[:30, b, :], identity=identf[:30, :30])
            I_("act", "copy", out=us_[:, b, 0:30], in_=ps[:, 0:30], disjoint=True)
        for ti, (t0, nt) in enumerate(TT):
            psa, psb = nps(), nps()
            S.mm(psa[:, :nt], [(a_[:, kc, :], hT[:, kc, t0:t0 + nt]) for kc in range(KC)])
            S.mm(psb[:, :nt], [(b_[:, kc, :], hT[:, kc, t0:t0 + nt]) for kc in range(KC)])
            s_ = sg[ti % 2]
            I_("act", "activation", out=s_[:, :nt], in_=psb[:, :nt], func=AF.Sigmoid)
            if t0 < T:
                I_("dve", "tensor_tensor", out=u_[:, 30 + t0:30 + t0 + nt], in0=psa[:, :nt], in1=s_[:, :nt],
                   op=ALU.mult, disjoint=True)
            else:
                I_("dve", "tensor_tensor", out=us[:, :nt], in0=psa[:, :nt], in1=s_[:, :nt], op=ALU.mult)
                for b in range(4):
                    I_("dve", "tensor_copy", out=us_[:, b, 30:34], in_=us[:, b * 16:b * 16 + 4], disjoint=True)
        ps = nps()
        I_("pe", "transpose", out=ps[0:30, 0:128], in_=u_[:, T:T + 30], identity=identf.v)
        I_("act", "copy", out=cpo[cc % 2].v, in_=ps[0:30, 0:128])
        Dm("sp", out=convp_out.v[:, cc * 128:(cc + 1) * 128], in_=cpo[cc % 2].v, disjoint=True)
        for b in range(4):
            ps = nps()
            I_("pe", "transpose", out=ps[0:4, 0:128], in_=us_[:, b, 30:34], identity=identf.v)
            I_("act", "copy", out=cso[b].v, in_=ps[0:4, 0:128])
            Dm("sp", out=convs_out.v[b, 26:30, cc * 128:(cc + 1) * 128], in_=cso[b].v, disjoint=True)
        eng = "dve"
        a = acc[cc % 2]
        as_ = accs[cc % 2]
        dg_ = dg[cc % 2]
        for j in range(31):
            I_("pool", "tensor_scalar", out=dg_[:, j, :], in0=L["identb"].v, scalar1=wdw[:, cc, j:j + 1], scalar2=None,
               op0=ALU.mult, disjoint=True)
        I_("act", "copy", out=ub.v, in_=u_.v)
        for (t0, nt) in TT[:4]:
            ps = nps()
            S.mm(ps[:, :nt], [(dg_[:, j, :], ub[:, j + t0:j + t0 + nt]) for j in range(31)])
            I_("act", "activation", out=a[:, t0:t0 + nt], in_=ps[:, :nt], func=AF.Identity, bias=bdw[:, cc:cc + 1],
               disjoint=True)
        I_(eng, "tensor_scalar", out=as_.v, in0=us_[:, :, 0:4], scalar1=wdw[:, cc, 0:1], scalar2=bdw[:, cc:cc + 1],
           op0=ALU.mult, op1=ALU.add)
        for j in range(1, 31):
            I_(eng, "scalar_tensor_tensor", out=as_.v, in0=us_[:, :, j:j + 4], scalar=wdw[:, cc, j:j + 1], in1=as_.v,
               op0=ALU.mult, op1=ALU.add)
        I_("act", "copy", out=ybf[:, cc, 0:T], in_=a[:, 0:T], disjoint=True)
        for b in range(4):
            I_("act", "copy", out=ybf[:, cc, T + b * 16:T + b * 16 + 4], in_=as_[:, b, :], disjoint=True)
        for ti, (t0, nt) in enumerate(TT):
            if t0 < T:
                src = a[:, t0:t0 + nt]
            else:
                I_("dve", "tensor_copy", out=us[:, :nt], in_=ybf[:, cc, t0:t0 + nt])
                src = us[:, :nt]
            q_ = sq[ti % 2]
            I_("act", "activation", out=q_[:, :nt], in_=src, func=AF.Square)
            p1, p2 = nps(), nps()
            I_("pe", "matmul", out=p1[:, :nt], lhsT=onesf.v, rhs=src, start=True, stop=True)
            I_("pe", "matmul", out=p2[:, :nt], lhsT=onesf.v, rhs=q_[:, :nt], start=True, stop=True)
            if cc == 0:
                I_("dve", "tensor_copy", out=st1[:, t0:t0 + nt], in_=p1[:, :nt], disjoint=True)
                I_("dve", "tensor_copy", out=st2[:, t0:t0 + nt], in_=p2[:, :nt], disjoint=True)
            else:
                I_("dve", "tensor_tensor", out=st1[:, t0:t0 + nt], in0=st1[:, t0:t0 + nt], in1=p1[:, :nt], op=ALU.add)
                I_("dve", "tensor_tensor", out=st2[:, t0:t0 + nt], in0=st2[:, t0:t0 + nt], in1=p2[:, :nt], op=ALU.add)
    tmp = acc[0]
    I_("dve", "tensor_scalar", out=st1.v, in0=st1.v, scalar1=1.0 / 1024, scalar2=None, op0=ALU.mult)
    I_("dve", "tensor_tensor", out=tmp[:, 0:NT - 64], in0=st1[:, 0:NT - 64], in1=st1[:, 0:NT - 64], op=ALU.mult)
    I_("dve", "scalar_tensor_tensor", out=st2[:, 0:NT - 64], in0=st2[:, 0:NT - 64], scalar=1.0 / 1024,
       in1=tmp[:, 0:NT - 64], op0=ALU.mult, op1=ALU.subtract)
    I_("dve", "tensor_tensor", out=us.v, in0=st1[:, T:NT], in1=st1[:, T:NT], op=ALU.mult)
    I_("dve", "scalar_tensor_tensor", out=st2[:, T:NT], in0=st2[:, T:NT], scalar=1.0 / 1024,
       in1=us.v, op0=ALU.mult, op1=ALU.subtract)
    I_("dve", "tensor_scalar", out=st2.v, in0=st2.v, scalar1=EPS, scalar2=None, op0=ALU.add)
    I_("act", "sqrt", out=st2.v, in_=st2.v)
    I_("dve", "reciprocal", out=st2.v, in_=st2.v)
    tn = acc[1]
    so_ = [S.sb("so%d" % i, [128, NT], BF16) for i in range(1)]
    for cc in range(8):
        I_("dve", "tensor_tensor", out=tn.v, in0=ybf[:, cc, :], in1=st1.v, op=ALU.subtract)
        I_("dve", "tensor_tensor", out=tn.v, in0=tn.v, in1=st2.v, op=ALU.mult)
        o = so_[0]
        I_("act", "activation", out=o.v, in_=tn.v, func=AF.Silu, scale=lng[:, cc:cc + 1], bias=lnb[:, cc:cc + 1])
        Dm("sp", out=sT_d.v[:, cc, :], in_=o.v, disjoint=True)


def finish(nc, S, outs):
    S.barrier()
    return nc


def _consts():
    bf = ml_dtypes.bfloat16
    ident = np.eye(128, dtype=np.float32)
    r = np.arange(128)
    cbP = np.where(r[None, :] <= r[:, None], 0.0, NEG).astype(np.float32)
    cbS = np.full((16, 128), NEG, np.float32)
    for row in range(16):
        q = row % 4
        for qq in range(q + 1):
            cbS[row, qq] = 0.0
    triu = (r[:, None] < r[None, :]).astype(np.float32)
    eoff = np.tile((np.arange(NEXP) * CAP).astype(np.float32)[None, :], (128, 1))
    vmask = (np.arange(64) % 16 < 4).astype(np.float32)[:, None]
    return {"c_identb": ident.astype(bf), "c_identf": ident, "c_cbP": cbP, "c_cbS": cbS,
            "c_triu": triu.astype(bf), "c_eoff": eoff, "c_vmask": vmask,
            "c_selB": (np.arange(16)[None, :] % 4 == np.arange(4)[:, None]).astype(np.float32)}


def make_in_maps(inp, cores=range(8)):
    f = lambda a: np.ascontiguousarray(a)
    shared = {
        "w_ada": inp["w_ada"][0], "b_ada": inp["b_ada"][0][None, :],
        "g_mix_pre": inp["g_mix_pre"], "g_mix_post": inp["g_mix_post"],
        "w_in": inp["w_in"][0], "w_oa": inp["w_oa"][0],
        "wdwT": f(inp["w_dw"][0].T.reshape(8, 128, 31).transpose(1, 0, 2)),
        "bdwT": f(inp["b_dw"][0].reshape(8, 128).T), "lngT": f(inp["ln_conv_g"][0].reshape(8, 128).T),
        "lnbT": f(inp["ln_conv_b"][0].reshape(8, 128).T),
        "w_pw2": inp["w_pw2"][0], "w_o": inp["w_o"][0],
        "g_ffn_pre": inp["g_ffn_pre"], "g_ffn_post": inp["g_ffn_post"],
        "w_router": inp["w_router"][0], "b_router": inp["b_router"],
        "w_eg": np.concatenate([inp["w_exp_gate"][0], inp["w_sh_gate"]], 0),
        "w_eu": np.concatenate([inp["w_exp_up"][0], inp["w_sh_up"]], 0),
        "w_ed": np.concatenate([inp["w_exp_down"][0], inp["w_sh_down"]], 0),
        "cache_k": inp["cache_k"][0].reshape(2560, -1), "cache_v": inp["cache_v"][0].reshape(2560, -1),
        "cache_ki": inp["cache_kidx"][0].reshape(2560, -1),
    }
    shared.update(_consts())
    shared = {k: f(np.asarray(v)) for k, v in shared.items()}
    maps = []
    for c in cores:
        xs = inp["x_sample"][4 * c:4 * c + 4]
        xs_v = np.broadcast_to(xs[:, None, :, :], (4, 4, 4, D)).reshape(64, D)
        xin = np.concatenate([inp["x_prompt"][c], xs_v], 0)
        cc = np.concatenate([inp["c_prompt"][c:c + 1], inp["c_sample"][4 * c:4 * c + 4]], 0)
        cT = f(cc.T.reshape(KC, 128, 5).transpose(1, 0, 2))
        m = dict(shared)
        m.update({"xin": f(xin), "cT": cT, "state_conv": f(inp["state_conv"][0, 4 * c:4 * c + 4]),
                  "ptT": f(inp["page_table"][4 * c:4 * c + 4].T.astype(np.int32))})
        maps.append(m)
    return maps


_NC_CACHE = {}


def kernel(**inputs):
    inp = {k: np.asarray(v) for k, v in inputs.items()}
    if "nc" not in _NC_CACHE:
        _NC_CACHE["nc"] = build()
    nc = _NC_CACHE["nc"]
    maps = make_in_maps(inp)
    res = run_bass_kernel_spmd(nc, maps, core_ids=list(range(8)))
    R = res.results
    B = 8
    y_p = np.stack([R[c]["y_out"][:T] for c in range(B)])
    sel = lambda a: a[T:].reshape(4, 4, 4, -1)[:, 0]
    y_s = np.concatenate([sel(R[c]["y_out"]) for c in range(B)], 0)
    k_p = np.stack([R[c]["k_out"][:T] for c in range(B)]).reshape(1, B, T, 2, 128)
    v_p = np.stack([R[c]["v_out"][:T] for c in range(B)]).reshape(1, B, T, 2, 128)
    i_p = np.stack([R[c]["ki_out"][:T] for c in range(B)]).reshape(1, B, T, 64)
    c_p = np.stack([R[c]["convp_out"] for c in range(B)]).reshape(1, B, 30, 1024)
    k_s = np.concatenate([sel(R[c]["k_out"]) for c in range(B)], 0).reshape(1, 32, 4, 2, 128)
    v_s = np.concatenate([sel(R[c]["v_out"]) for c in range(B)], 0).reshape(1, 32, 4, 2, 128)
    i_s = np.concatenate([sel(R[c]["ki_out"]) for c in range(B)], 0).reshape(1, 32, 4, 64)
    c_s = np.concatenate([R[c]["convs_out"] for c in range(B)], 0).reshape(1, 32, 30, 1024)
    return tuple(np.ascontiguousarray(a, dtype=np.float32) for a in
                 (y_p, y_s, k_p, v_p, i_p, c_p, k_s, v_s, i_s, c_s))


def gather_prep(nc, S, L):
    I_, Dm = S.I, S.D
    ptb = S.sb("ptb", [64, 4], I32)
    Dm("sp", out=ptb.v, in_=L["ptT"].v)
    gb = [S.sb("gb%d" % i, [64, 8192], F32) for i in range(2)]
    ptf = S.sb("ptf", [64, 4], F32)
    I_("dve", "tensor_copy", out=ptf.v, in_=ptb.v)
    pcf = S.sb("pcf", [64, 4, 4], F32)
    pci = S.sb("pci", [64, 4, 4], I32)
    for ch in range(4):
        I_("dve", "tensor_scalar", out=pcf[:, :, ch], in0=ptf.v, scalar1=4.0, scalar2=float(ch),
           op0=ALU.mult, op1=ALU.add, disjoint=True)
    I_("dve", "tensor_copy", out=pci.v, in_=pcf.v)
    thunks = []
    gi = [0]
    for b in range(4):
        for (cache, dst, w) in ((L["cache_k"], L["Ks_d"], 256), (L["cache_v"], L["Vs_d"], 256),
                                (L["cache_ki"], L["kis_d"], 64)):
            spc = 8192 // w
            nch = 128 // spc
            for ch in range(nch):
                def th(b=b, cache=cache, dst=dst, w=w, spc=spc, nch=nch, ch=ch):
                    g = gb[gi[0] % 2]
                    gi[0] += 1
                    if nch == 1:
                        src, off, bc_ = cache.v, ptb[:, b:b + 1], 2559
                    else:
                        src, off, bc_ = cache.v.rearrange("p (c x) -> (p c) x", c=nch), pci[:, b, ch:ch + 1], 2560 * nch - 1
                    Dm("pool", indirect=True, out=g.v, out_offset=None, in_=src, in_offset=(off, 0),
                       bounds_check=bc_, oob_is_err=False)
                    dv = dst.v[b, 0:8192, :].rearrange("(j s) d -> j s d", s=128)[:, ch * spc:(ch + 1) * spc, :]
                    Dm("sp", out=dv, in_=g.v.rearrange("j (s d) -> j s d", d=w), disjoint=True)
                thunks.append(th)
    return thunks


def gather_stage(nc, S, L):
    I_, Dm = S.I, S.D
    with S.scope():
        zt = S.sb("zt", [112, 256], F32)
        I_("dve", "memset", ap=zt.v.ap, extra_writes=[zt], constant=0.0)
        for b in range(4):
            r0 = T + b * 16
            Dm("sp", out=L["Ks_d"].v[b, 8192:8208, :], in_=L["k_out"].v[r0:r0 + 16, :], disjoint=True, semof=L["Ks_d"])
            Dm("sp", out=L["Vs_d"].v[b, 8192:8208, :], in_=L["v_out"].v[r0:r0 + 16, :], disjoint=True, semof=L["Vs_d"])
            Dm("sp", out=L["kis_d"].v[b, 8192:8208, :], in_=L["ki_out"].v[r0:r0 + 16, :], disjoint=True, semof=L["kis_d"])
            Dm("sp", out=L["Ks_d"].v[b, 8208:NKS, :], in_=zt.v, disjoint=True)
            Dm("sp", out=L["Vs_d"].v[b, 8208:NKS, :], in_=zt.v, disjoint=True)
            Dm("sp", out=L["kis_d"].v[b, 8208:NKS, :], in_=zt[:, 0:64], disjoint=True)


def attn_stage(nc, S, L):
    I_, Dm = S.I, S.D
    nps, npb, PSO, PSI = L["nps"], L["npb"], L["PSO"], L["PSI"]
    identb, identf = L["identb"], L["identf"]
    SCALE = 128 ** -0.5
    with S.scope():
        KT = S.sb("KT", [128, 2, NKS], BF16)
        kiT2 = S.sb("kiT2", [128, NKS], BF16)
        V = S.sb("V", [128, NKS // 128, 256], BF16)
        Ib = S.sb("Ib", [128, NKS], F32)
        mb = S.sb("mb", [128, NKS], BF16)
        junkc = mb
        kin = [S.sb("kin%d" % i, [128, 4, 256], F32) for i in range(2)]
        vin = [S.sb("vin%d" % i, [128, 4, 256], F32) for i in range(2)]
        kiin = [S.sb("kiin%d" % i, [128, 4, 128], F32) for i in range(2)]
        wdg = [S.sb("wdg%d" % i, [128, 16, 128], BF16) for i in range(2)]
        qst = S.sb("qst", [128, 32], BF16)
        A3 = S.sb("A3", [4, 2, 8, 4], F32)
        selB = S.sb("selB", [4, 16], F32)
        Dm("sp", out=selB.v, in_=L["c_selB"].v)
        Wsel = [S.sb("Wsel%d" % i, [32, 16], BF16) for i in range(2)]
        rls = [S.sb("rls%d" % i, [32, 512], BF16) for i in range(4)]
        qTb = [S.sb("qTb%d" % i, [128, 8, 128], BF16) for i in range(2)]
        qiTb = [S.sb("qiTb%d" % i, [128, 8, 128], BF16) for i in range(2)]
        wib = [S.sb("wib%d" % i, [128, 16], F32) for i in range(2)]
        rl = [S.sb("rl%d" % i, [128, 512], BF16) for i in range(4)]
        Sm = [S.sb("Sm%d" % i, [128, 512], F32) for i in range(2)]
        P = [S.sb("P%d" % i, [128, 512], BF16) for i in range(2)]
        PT = [S.sb("PT%d" % i, [128, 4, 128], BF16) for i in range(2)]
        rsum = S.sb("rsum", [128, 20], F32)
        sm = {k: S.sb("sm_" + k, [128, 1], F32) for k in ("lo", "hi", "w0", "mid", "cnt", "step", "thr", "rs", "rinv")}
        Osb = S.sb("Osb", [128, 128], BF16)
        aoTb = [S.sb("aoTb%d" % i, [128, 8, 128], BF16) for i in range(2)]
        aoTs = S.sb("aoTs", [128, 8, 64], BF16)
        qvs = [S.sb("qvs%d" % i, [128, 16], BF16) for i in range(2)]
        cbP = S.sb("cbP", [128, 128], F32)
        cbS = S.sb("cbS", [16, 128], F32)
        Dm("sp", out=cbP.v, in_=L["c_cbP"].v)
        Dm("sp", out=cbS.v, in_=L["c_cbS"].v)
        I_("dve", "memset", ap=aoTs.v.ap, extra_writes=[aoTs], constant=0.0)

        def load_seq(Ksrc, Vsrc, kisrc, nblk):
            for g0 in range(0, nblk, 4):
                ng = min(4, nblk - g0)
                k_, v_, i_ = kin[(g0 // 4) % 2], vin[(g0 // 4) % 2], kiin[(g0 // 4) % 2]
                rows = slice(g0 * 128, (g0 + ng) * 128)
                Dm("sp", out=k_[:, 0:ng, :], in_=Ksrc[rows, :].rearrange("(j p) d -> p j d", p=128))
                Dm("sp", out=v_[:, 0:ng, :], in_=Vsrc[rows, :].rearrange("(j p) d -> p j d", p=128))
                Dm("sp", out=i_[:, 0:ng, 0:64], in_=kisrc[rows, :].rearrange("(j p) d -> p j d", p=128))
                Dm("sp", out=i_[:, 0:ng, 64:128], in_=kisrc[rows, :].rearrange("(j p) d -> p j d", p=128), disjoint=True)
                I_("pool", "tensor_copy", out=V[:, g0:g0 + ng, :], in_=v_[:, 0:ng, :], disjoint=True)
                for kvh in range(2):
                    ps = nps()
                    for j in range(ng):
                        I_("pe", "transpose", out=ps[:, j * 128:(j + 1) * 128], in_=k_[:, j, kvh * 128:(kvh + 1) * 128],
                           identity=identf.v, disjoint=True)
                    I_("act", "copy", out=KT[:, kvh, g0 * 128:(g0 + ng) * 128], in_=ps[:, 0:ng * 128], disjoint=True)
                ps = nps()
                for j in range(ng):
                    I_("pe", "transpose", out=ps[:, j * 128:(j + 1) * 128], in_=i_[:, j, :], identity=identf.v, disjoint=True)
                I_("dve", "tensor_copy", out=kiT2[:, g0 * 128:(g0 + ng) * 128], in_=ps[:, 0:ng * 128], disjoint=True)

        cnt_ = [0]
        L["nrot"][0] = 3
        PSI2 = [PSI, L["PS"][3]]
        IbP = [Buf("IbP%d" % i, Ib.t[:, i * 2048:(i + 1) * 2048], "sb") for i in range(2)]
        mbP = [Buf("mbP%d" % i, mb.t[:, i * 2048:(i + 1) * 2048], "sb") for i in range(2)]

        def index(R, nk, qiTv, wiv, IbX, stacked=False):
            nkt = (nk + 511) // 512
            pi = [0]
            if not stacked:
                wd_ = wdg[cnt_[0] % 2]
                cnt_[0] += 1
                for h in range(16):
                    I_("pool", "tensor_scalar", out=wd_[:R, h, :R], in0=identb[:R, :R], scalar1=wiv[:R, h:h + 1],
                       scalar2=None, op0=ALU.mult, disjoint=True)
                ri = [0]

                def dots(kt, h):
                    k0 = kt * 512
                    kn = min(512, nk - k0)
                    hp, half = h // 2, h % 2
                    ps = nps()
                    I_("pe", "matmul", out=ps[:R, :kn], lhsT=qiTv[half * 64:(half + 1) * 64, hp, :R],
                       rhs=kiT2[half * 64:(half + 1) * 64, k0:k0 + kn], start=True, stop=True)
                    r_ = rl[ri[0] % 4]
                    ri[0] += 1
                    I_("act", "activation", out=r_[:R, :kn], in_=ps[:R, :kn], func=AF.Relu)
                    return r_

                def acc(kt, h, r_):
                    k0 = kt * 512
                    kn = min(512, nk - k0)
                    pI = PSI2[kt % 2]
                    I_("pe", "matmul", out=pI[:R, :kn], lhsT=wd_[:R, h, :R], rhs=r_[:R, :kn], start=(h == 0),
                       stop=(h == 15), inc=(h == 15))
                    if h == 15:
                        I_("dve", "tensor_copy", out=IbX[:R, k0:k0 + kn], in_=pI[:R, :kn], disjoint=True)

                pend = None
                for kt in range(nkt):
                    for h in range(16):
                        r_ = dots(kt, h)
                        if pend is not None:
                            acc(*pend)
                        pend = (kt, h, r_)
                acc(*pend)
            else:
                I_("dve", "tensor_copy", out=qst.v.rearrange("p (h q) -> p h q", q=4), in_=qiTv[:, :, 0:4])
                for half in range(2):
                    for qq in range(4):
                        I_("dve", "tensor_scalar", out=A3[:, half, :, qq], in0=wiv[0:4, half:16:2],
                           scalar1=identf[0:4, qq:qq + 1], scalar2=None, op0=ALU.mult, disjoint=True)
                    ps = nps()
                    I_("pe", "matmul", out=ps[0:32, 0:16], lhsT=A3[:, half, :, :].rearrange("p h q -> p (h q)"),
                       rhs=selB.v, start=True, stop=True)
                    I_("dve", "tensor_copy", out=Wsel[half].v, in_=ps[0:32, 0:16])
                ri = 0
                pend = None

                def sacc(kt, rr):
                    k0 = kt * 512
                    kn = min(512, nk - k0)
                    pI = PSI2[kt % 2]
                    for half in range(2):
                        I_("pe", "matmul", out=pI[:R, :kn], lhsT=Wsel[half].v, rhs=rr[half][:, :kn], start=(half == 0),
                           stop=(half == 1), inc=(half == 1))
                    I_("dve", "tensor_copy", out=IbX[:R, k0:k0 + kn], in_=pI[:R, :kn], disjoint=True)

                for kt in range(nkt):
                    k0 = kt * 512
                    kn = min(512, nk - k0)
                    rr = []
                    for half in range(2):
                        ps = nps()
                        I_("pe", "matmul", out=ps[0:32, :kn], lhsT=qst[half * 64:(half + 1) * 64, :],
                           rhs=kiT2[half * 64:(half + 1) * 64, k0:k0 + kn], start=True, stop=True)
                        r_ = rls[ri % 4]
                        ri += 1
                        I_("act", "activation", out=r_[:, :kn], in_=ps[0:32, :kn], func=AF.Relu)
                        rr.append(r_)
                    if pend is not None:
                        sacc(*pend)
                    pend = (kt, rr)
                sacc(*pend)

        def thresh(R, nk, IbX, mbX, cb, bisect):
            lo, hi, w0, mid, cnt, step, thr = (sm[k] for k in ("lo", "hi", "w0", "mid", "cnt", "step", "thr"))
            if bisect:
                I_("dve", "tensor_reduce", out=hi[:R, :], in_=IbX[:R, :nk], axis=AX.X, op=ALU.max)
                I_("dve", "tensor_reduce", out=lo[:R, :], in_=IbX[:R, :nk], axis=AX.X, op=ALU.min)
                I_("dve", "tensor_tensor", out=w0[:R, :], in0=hi[:R, :], in1=lo[:R, :], op=ALU.subtract)
            I_("dve", "tensor_tensor", out=IbX[:R, nk - 128:nk], in0=IbX[:R, nk - 128:nk], in1=cb[:R, :], op=ALU.add)
            if bisect:
                for it in range(BIS_ITERS):
                    f = 2.0 ** -(it + 1)
                    I_("dve", "scalar_tensor_tensor", out=mid[:R, :], in0=w0[:R, :], scalar=f, in1=lo[:R, :],
                       op0=ALU.mult, op1=ALU.add)
                    I_("dve", "tensor_scalar", out=mbX[:R, :nk], in0=IbX[:R, :nk], scalar1=mid[:R, 0:1], scalar2=0.0,
                       op0=ALU.is_ge, op1=ALU.add, accum_out=cnt[:R, :])
                    I_("dve", "tensor_scalar", out=step[:R, :], in0=cnt[:R, :], scalar1=255.5, scalar2=w0[:R, 0:1],
                       op0=ALU.is_ge, op1=ALU.mult)
                    I_("dve", "scalar_tensor_tensor", out=lo[:R, :], in0=step[:R, :], scalar=f, in1=lo[:R, :],
                       op0=ALU.mult, op1=ALU.add)
                thr_v = lo
            else:
                I_("dve", "memset", ap=thr[:R, :].ap, extra_writes=[thr], constant=-1.0e29)
                thr_v = thr
            I_("dve", "tensor_scalar", out=mbX[:R, :nk], in0=IbX[:R, :nk], scalar1=thr_v[:R, 0:1], scalar2=MNEG,
               op0=ALU.is_lt, op1=ALU.mult)

        def run_passes(R, nk, mbX, passes):
            nkt = (nk + 511) // 512
            nblk = nk // 128
            for (qv, kvh, out_fn) in passes:
                bi = [0]

                def emit_S(kt):
                    k0 = kt * 512
                    kn = min(512, nk - k0)
                    ps = nps()
                    I_("pe", "matmul", out=ps[:R, :kn], lhsT=qv, rhs=KT[:, kvh, k0:k0 + kn], start=True, stop=True)
                    s_, p_ = Sm[kt % 2], P[kt % 2]
                    I_("dve", "scalar_tensor_tensor", out=s_[:R, :kn], in0=ps[:R, :kn], scalar=SCALE,
                       in1=mbX[:R, k0:k0 + kn], op0=ALU.mult, op1=ALU.add)
                    I_("act", "activation", out=p_[:R, :kn], in_=s_[:R, :kn], func=AF.Exp,
                       accum_out=rsum[:R, kt:kt + 1], disjoint=True)
                    return p_

                def emit_PV(kt, p_):
                    k0 = kt * 512
                    kn = min(512, nk - k0)
                    nj = kn // 128
                    t_ = PT[kt % 2]
                    pb = npb()
                    for j in range(nj):
                        I_("pe", "transpose", out=pb[:, j * 128:j * 128 + R], in_=p_[:R, j * 128:(j + 1) * 128],
                           identity=identb[:R, :R], disjoint=True)
                    I_("act", "copy", out=t_[:, 0:nj, :R],
                       in_=pb[:, 0:nj * 128].rearrange("p (j r) -> p j r", r=128)[:, :, :R])
                    for j in range(nj):
                        I_("pe", "matmul", out=PSO[:R, 0:128], lhsT=t_[:, j, :R],
                           rhs=V[:, kt * 4 + j, kvh * 128:(kvh + 1) * 128], start=(bi[0] == 0),
                           stop=(bi[0] == nblk - 1), inc=(bi[0] == nblk - 1))
                        bi[0] += 1

                pend = None
                for kt in range(nkt):
                    p_ = emit_S(kt)
                    if pend is not None:
                        emit_PV(*pend)
                    pend = (kt, p_)
                emit_PV(*pend)
                I_("dve", "tensor_reduce", out=sm["rs"][:R, :], in_=rsum[:R, :nkt], axis=AX.X, op=ALU.add)
                I_("dve", "reciprocal", out=sm["rinv"][:R, :], in_=sm["rs"][:R, :])
                I_("act", "activation", out=Osb[:R, :], in_=PSO[:R, 0:128], func=AF.Copy, scale=sm["rinv"][:R, 0:1])
                pb = npb()
                I_("pe", "transpose", out=pb[:, 0:R], in_=Osb[:R, :], identity=identb[:R, :R])
                out_fn(pb)

        load_seq(L["k_out"].v, L["v_out"].v, L["ki_out"].v, 16)

        def qload(j):
            q0 = j * 128
            Dm("sp", out=qTb[j % 2].v, in_=L["qT_d"].v[:, :, q0:q0 + 128])
            Dm("sp", out=qiTb[j % 2].v, in_=L["qiT_d"].v[:, :, q0:q0 + 128])
            Dm("sp", out=wib[j % 2].v, in_=L["wi_d"].v[q0:q0 + 128, :])

        qload(0)
        index(128, 128, qiTb[0], wib[0], IbP[0])
        for j in range(16):
            q0 = j * 128
            qT_, ao_ = qTb[j % 2], aoTb[j % 2]
            thresh(128, (j + 1) * 128, IbP[j % 2], mbP[j % 2], cbP, bisect=(j >= 2))
            if j + 1 < 16:
                qload(j + 1)
                index(128, (j + 2) * 128, qiTb[(j + 1) % 2], wib[(j + 1) % 2], IbP[(j + 1) % 2])
            passes = []
            for h in range(8):
                def ofn(pb, h=h, ao_=ao_):
                    I_("dve", "tensor_copy", out=ao_[:, h, :], in_=pb[:, 0:128], disjoint=True)
                passes.append((qT_[:, h, :], h // 4, ofn))
            run_passes(128, (j + 1) * 128, mbP[j % 2], passes)
            Dm("sp", out=L["aoT_d"].v[:, :, q0:q0 + 128], in_=ao_.v, disjoint=True)
        S.barrier()
        for b in range(4):
            load_seq(L["Ks_d"].v[b], L["Vs_d"].v[b], L["kis_d"].v[b], NKS // 128)
            c0 = T + b * 16
            qT_, qiT_, wi_ = qTb[b % 2], qiTb[b % 2], wib[b % 2]
            Dm("sp", out=qT_[:, :, 0:16], in_=L["qT_d"].v[:, :, c0:c0 + 16])
            Dm("sp", out=qiT_[:, :, 0:16], in_=L["qiT_d"].v[:, :, c0:c0 + 16])
            Dm("sp", out=wi_[0:16, :], in_=L["wi_d"].v[c0:c0 + 16, :])
            passes = []
            for kvh in range(2):
                qv = qvs[kvh]
                for h4 in range(4):
                    I_("dve", "tensor_copy", out=qv[:, h4 * 4:h4 * 4 + 4], in_=qT_[:, kvh * 4 + h4, h4 * 4:h4 * 4 + 4],
                       disjoint=True)

                def ofn(pb, kvh=kvh, b=b):
                    for h4 in range(4):
                        I_("dve", "tensor_copy", out=aoTs[:, kvh * 4 + h4, b * 16:b * 16 + 4],
                           in_=pb[:, h4 * 4:h4 * 4 + 4], disjoint=True)
                passes.append((qv.v, kvh, ofn))
            index(16, NKS, qiT_, wi_, Ib, stacked=True)
            thresh(16, NKS, Ib, mb, cbS, True)
            run_passes(16, NKS, mb, passes)
        Dm("sp", out=L["aoT_d"].v[:, :, T:NT], in_=aoTs.v, disjoint=True)
        L["nrot"][0] = 4


def merge_stage(nc, S, L):
    I_, Dm = S.I, S.D
    nps, npb, wview = L["nps"], L["npb"], L["wview"]
    w_in = L["w_in"]
    with S.scope():
        hTt = S.sb("hTt", [128, KC, 512], BF16)
        aoTt = S.sb("aoTt", [128, 8, 512], BF16)
        sTt = S.sb("sTt", [128, 8, 512], BF16)
        mTt = S.sb("mTt", [128, KC, 512], BF16)
        wga = [S.sb("wga%d" % i, [128, KC, 128], BF16) for i in range(2)]
        wgb = [S.sb("wgb%d" % i, [128, KC, 128], BF16) for i in range(2)]
        woa = [S.sb("woa%d" % i, [128, 8, 128], BF16) for i in range(2)]
        wpw = [S.sb("wpw%d" % i, [128, 8, 128], BF16) for i in range(2)]
        sga = [S.sb("sga%d" % i, [128, 512], F32) for i in range(2)]
        sgb = [S.sb("sgb%d" % i, [128, 512], F32) for i in range(2)]
        wi_ = 0

        def load_w(i):
            fc = i % 16
            Dm("pool", out=wga[i % 2].v, in_=wview(w_in, 4688 + fc * 128, 128))
            Dm("pool", out=wgb[i % 2].v, in_=wview(w_in, 6736 + fc * 128, 128))
            Dm("pool", out=woa[i % 2].v, in_=wview(L["w_oa"], fc * 128, 128, kc=8))
            Dm("pool", out=wpw[i % 2].v, in_=wview(L["w_pw2"], fc * 128, 128, kc=8))

        load_w(0)
        for (t0, nt) in TT:
            Dm("sp", out=hTt[:, :, :nt], in_=L["hT_d"].v[:, :, t0:t0 + nt])
            Dm("sp", out=aoTt[:, :, :nt], in_=L["aoT_d"].v[:, :, t0:t0 + nt])
            Dm("sp", out=sTt[:, :, :nt], in_=L["sT_d"].v[:, :, t0:t0 + nt])
            for fc in range(16):
                a, b_, o, p = wga[wi_ % 2], wgb[wi_ % 2], woa[wi_ % 2], wpw[wi_ % 2]
                sa, sb_ = sga[wi_ % 2], sgb[wi_ % 2]
                wi_ += 1
                if wi_ < 16 * len(TT):
                    load_w(wi_)
                pga, pA, pgb, pB = nps(), nps(), nps(), nps()
                S.mm(pga[:, :nt], [(a[:, kc, :], hTt[:, kc, :nt]) for kc in range(KC)])
                S.mm(pA[:, :nt], [(o[:, kc, :], aoTt[:, kc, :nt]) for kc in range(8)])
                S.mm(pgb[:, :nt], [(b_[:, kc, :], hTt[:, kc, :nt]) for kc in range(KC)])
                S.mm(pB[:, :nt], [(p[:, kc, :], sTt[:, kc, :nt]) for kc in range(8)])
                I_("act", "activation", out=sa[:, :nt], in_=pga[:, :nt], func=AF.Sigmoid)
                I_("dve", "tensor_tensor", out=sa[:, :nt], in0=sa[:, :nt], in1=pA[:, :nt], op=ALU.mult)
                I_("act", "activation", out=sb_[:, :nt], in_=pgb[:, :nt], func=AF.Sigmoid)
                I_("dve", "tensor_tensor", out=sb_[:, :nt], in0=sb_[:, :nt], in1=pB[:, :nt], op=ALU.mult)
                I_("dve", "tensor_tensor", out=mTt[:, fc, :nt], in0=sa[:, :nt], in1=sb_[:, :nt], op=ALU.add, disjoint=True)
            Dm("sp", out=L["mT_d"].v[:, :, t0:t0 + nt], in_=mTt[:, :, :nt], disjoint=True)

    with S.scope():
        wo = S.sb("wo", [128, KC, D], BF16)
        for g in range(4):
            Dm("pool", out=wo[:, :, g * 512:(g + 1) * 512], in_=wview(L["w_o"], g * 512, 512), disjoint=True)
        wr = S.sb("wr", [128, KC, NEXP], BF16)
        Dm("pool", out=wr.v, in_=wview(L["w_router"], 0, NEXP))
        brt = S.sb("brt", [128, NEXP], F32)
        L["load_bc"](brt, L["b_router"].v[0:1, :], 128)
        triu = S.sb("triu", [128, 128], BF16)
        Dm("sp", out=triu.v, in_=L["c_triu"].v)
        eoff = S.sb("eoff", [128, NEXP], F32)
        Dm("sp", out=eoff.v, in_=L["c_eoff"].v)
        vmask = S.sb("vmask", [64, 1], F32)
        Dm("sp", out=vmask.v, in_=L["c_vmask"].v)
        carry = S.sb("carry", [128, NEXP], F32)
        I_("dve", "memset", ap=carry.v.ap, extra_writes=[carry], constant=0.0)
        osb = S.sb("osb", [128, D], F32)
        gtmp = osb
        g1 = [S.sb("g1_%d" % i, [128, D], F32) for i in range(2)]
        gs2 = [S.sb("gs2_%d" % i, [128, D], F32) for i in range(2)]
        sh2 = [S.sb("sh2_%d" % i, [128, D], F32) for i in range(2)]
        for s in range(2):
            nr = 128 if s == 0 else 64
            L["load_mod"](g1[s], 2, s == 1)
            L["load_bc"](gtmp, L["g_mix_post"].v[0:1, :], 128)
            I_("dve", "tensor_tensor", out=g1[s][:nr, :], in0=g1[s][:nr, :], in1=gtmp[:nr, :], op=ALU.mult)
            L["load_mod"](gs2[s], 4, s == 1)
            L["load_bc"](gtmp, L["g_ffn_pre"].v[0:1, :], 128)
            I_("dve", "scalar_tensor_tensor", out=gs2[s][:nr, :], in0=gs2[s][:nr, :], scalar=1.0, in1=gtmp[:nr, :],
               op0=ALU.add, op1=ALU.mult)
            L["load_mod"](sh2[s], 3, s == 1)
        mTr = [S.sb("mTr%d" % i, [128, KC, 128], BF16) for i in range(2)]
        xt = [S.sb("xt%d" % i, [128, D], F32) for i in range(1)]
        x1 = [S.sb("x1_%d" % i, [128, D], F32) for i in range(1)]
        junk = S.sb("junk", [128, D], BF16)
        h2b = [S.sb("h2b%d" % i, [128, D], BF16) for i in range(2)]
        h2Tt = [S.sb("h2Tt%d" % i, [128, KC, 128], BF16) for i in range(2)]
        ss = S.sb("ss", [128, 1], F32)
        rs = S.sb("rs", [128, 1], F32)
        sc = S.sb("sc", [128, NEXP], F32)
        sel = S.sb("sel", [128, NEXP], F32)
        selm = S.sb("selm", [128, NEXP], F32)
        g8 = S.sb("g8", [128, 8, 8], F32)
        gsc = S.sb("gsc", [128, 8], F32)
        m8 = S.sb("m8", [128, 8], F32)
        pen = S.sb("pen", [128, 8], F32)
        e8 = S.sb("e8", [128, 8], F32)
        em = S.sb("em", [128, NEXP], F32)
        emb = S.sb("emb", [128, NEXP], BF16)
        gw = S.sb("gw", [128, NEXP], F32)
        den = S.sb("den", [128, 1], F32)
        pos = S.sb("pos", [128, NEXP], F32)
        ok = S.sb("ok", [128, NEXP], F32)
        key = S.sb("key", [128, NEXP], F32)
        d8 = S.sb("d8", [128, 8], F32)
        neg = S.sb("neg", [128, 8], F32)
        oh = S.sb("oh", [128, NEXP], F32)
        idx8, w8 = L["idx8"], L["w8"]
        for ti, (r0, nr) in enumerate(RT):
            s = 1 if r0 >= T else 0
            m_, x_, x1_, hb, hT_ = mTr[ti % 2], xt[0], x1[0], h2b[ti % 2], h2Tt[ti % 2]
            Dm("sp", out=m_[:, :, :nr], in_=L["mT_d"].v[:, :, r0:r0 + nr])
            Dm("sp", out=x_[:nr, :], in_=L["xin"].v[r0:r0 + nr, :])
            for cg in range(4):
                ps = nps()
                S.mm(ps[:nr, :], [(m_[:, kc, :nr], wo[:, kc, cg * 512:(cg + 1) * 512]) for kc in range(KC)])
                I_("act", "copy", out=osb[:nr, cg * 512:(cg + 1) * 512], in_=ps[:nr, :], disjoint=True)
            I_("act", "activation", out=junk[:nr, :], in_=osb[:nr, :], func=AF.Square, accum_out=ss[:nr, :])
            L["rstd_from_ss"](rs, ss, nr, D)
            I_("dve", "scalar_tensor_tensor", out=osb[:nr, :], in0=osb[:nr, :], scalar=rs[:nr, 0:1], in1=g1[s][:nr, :],
               op0=ALU.mult, op1=ALU.mult)
            I_("dve", "tensor_tensor", out=x1_[:nr, :], in0=osb[:nr, :], in1=x_[:nr, :], op=ALU.add)
            Dm("sp", out=L["x1_d"].v[r0:r0 + nr, :], in_=x1_[:nr, :], disjoint=True)
            I_("act", "activation", out=junk[:nr, :], in_=x1_[:nr, :], func=AF.Square, accum_out=ss[:nr, :])
            L["rstd_from_ss"](rs, ss, nr, D)
            I_("dve", "scalar_tensor_tensor", out=osb[:nr, :], in0=x1_[:nr, :], scalar=rs[:nr, 0:1], in1=gs2[s][:nr, :],
               op0=ALU.mult, op1=ALU.mult)
            I_("dve", "tensor_tensor", out=hb[:nr, :], in0=osb[:nr, :], in1=sh2[s][:nr, :], op=ALU.add)
            L["transpose_to"](lambda c: hT_[:, c, :nr], hb, nr, KC)
            Dm("sp", out=L["h2T_d"].v[:, :, r0:r0 + nr], in_=hT_[:, :, :nr], disjoint=True)
            ps = nps()
            S.mm(ps[:nr, :NEXP], [(hT_[:, kc, :nr], wr[:, kc, :]) for kc in range(KC)])
            I_("act", "activation", out=sc[:nr, :], in_=ps[:nr, :NEXP], func=AF.Sigmoid)
            I_("dve", "tensor_tensor", out=sel[:nr, :], in0=sc[:nr, :], in1=brt[:nr, :], op=ALU.add)
            for g in range(8):
                I_("dve", "max", out=g8[:nr, g, :], in_=sel[:nr, g * 8:(g + 1) * 8], disjoint=True)
            I_("dve", "tensor_tensor", out=gsc[:nr, :], in0=g8[:nr, :, 0], in1=g8[:nr, :, 1], op=ALU.add)
            I_("dve", "max", out=m8[:nr, :], in_=gsc[:nr, :])
            I_("dve", "tensor_scalar", out=pen[:nr, :], in0=gsc[:nr, :], scalar1=m8[:nr, 3:4], scalar2=1.0e9,
               op0=ALU.is_lt, op1=ALU.mult)
            for g in range(8):
                I_("dve", "tensor_scalar", out=selm[:nr, g * 8:(g + 1) * 8], in0=sel[:nr, g * 8:(g + 1) * 8],
                   scalar1=pen[:nr, g:g + 1], scalar2=None, op0=ALU.subtract, disjoint=True)
            I_("dve", "max", out=e8[:nr, :], in_=selm[:nr, :])
            I_("dve", "tensor_scalar", out=em[:nr, :], in0=selm[:nr, :], scalar1=e8[:nr, 7:8], scalar2=None, op0=ALU.is_ge)
            I_("dve", "tensor_tensor", out=gw[:nr, :], in0=sc[:nr, :], in1=em[:nr, :], op=ALU.mult)
            I_("dve", "tensor_reduce", out=den[:nr, :], in_=gw[:nr, :], axis=AX.X, op=ALU.add)
            I_("dve", "reciprocal", out=den[:nr, :], in_=den[:nr, :])
            I_("dve", "tensor_scalar", out=gw[:nr, :], in0=gw[:nr, :], scalar1=den[:nr, 0:1], scalar2=2.5,
               op0=ALU.mult, op1=ALU.mult)
            if s == 1:
                I_("dve", "tensor_scalar", out=em[:nr, :], in0=em[:nr, :], scalar1=vmask[:nr, 0:1], scalar2=None,
                   op0=ALU.mult)
            I_("dve", "tensor_copy", out=emb[:nr, :], in_=em[:nr, :])
            ps = nps()
            I_("pe", "matmul", out=ps[:nr, :NEXP], lhsT=triu[:nr, :nr], rhs=emb[:nr, :], start=True, stop=True)
            I_("dve", "tensor_tensor", out=pos[:nr, :], in0=ps[:nr, :NEXP], in1=carry[:nr, :], op=ALU.add)
            ps2 = nps()
            I_("pe", "matmul", out=ps2[:, :NEXP], lhsT=L["onesb"][:nr, :], rhs=emb[:nr, :], start=True, stop=True)
            I_("dve", "tensor_tensor", out=carry.v, in0=carry.v, in1=ps2[:, :NEXP], op=ALU.add)
            I_("dve", "tensor_scalar", out=ok[:nr, :], in0=pos[:nr, :], scalar1=float(CAP) - 0.5, scalar2=None, op0=ALU.is_lt)
            I_("dve", "tensor_tensor", out=ok[:nr, :], in0=ok[:nr, :], in1=em[:nr, :], op=ALU.mult)
            I_("dve", "scalar_tensor_tensor", out=key[:nr, :], in0=pos[:nr, :], scalar=1.0, in1=eoff[:nr, :],
               op0=ALU.add, op1=ALU.add)
            I_("dve", "tensor_tensor", out=key[:nr, :], in0=key[:nr, :], in1=ok[:nr, :], op=ALU.mult)
            I_("dve", "tensor_scalar", out=key[:nr, :], in0=key[:nr, :], scalar1=-1.0, scalar2=None, op0=ALU.add)
            I_("dve", "max", out=d8[:nr, :], in_=key[:nr, :])
            I_("dve", "tensor_scalar", out=neg[:nr, :], in0=d8[:nr, :], scalar1=0.0, scalar2=1.0e7, op0=ALU.is_lt, op1=ALU.mult)
            I_("dve", "tensor_tensor", out=neg[:nr, :], in0=neg[:nr, :], in1=d8[:nr, :], op=ALU.add)
            I_("dve", "tensor_copy", out=idx8[:nr, ti, :], in_=neg[:nr, :], disjoint=True)
            for j in range(8):
                I_("dve", "tensor_scalar", out=oh[:nr, :], in0=key[:nr, :], scalar1=d8[:nr, j:j + 1], scalar2=None,
                   op0=ALU.is_equal)
                I_("dve", "tensor_tensor", out=oh[:nr, :], in0=oh[:nr, :], in1=gw[:nr, :], op=ALU.mult)
                I_("dve", "tensor_reduce", out=w8[:nr, ti, j:j + 1], in_=oh[:nr, :], axis=AX.X, op=ALU.add, disjoint=True)
            I_("dve", "tensor_scalar", out=neg[:nr, :], in0=d8[:nr, :], scalar1=0.0, scalar2=None, op0=ALU.is_ge)
            I_("dve", "tensor_tensor", out=w8[:nr, ti, :], in0=w8[:nr, ti, :], in1=neg[:nr, :], op=ALU.mult)
            for j in range(8):
                Dm("pool", indirect=True, out=L["Xs_d"].v, out_offset=(idx8[:nr, ti, j:j + 1], 0), in_=hb[:nr, :],
                   in_offset=None, bounds_check=NEXP * CAP - 1, oob_is_err=False, disjoint=True)


def expert_stage(nc, S, L):
    I_, Dm = S.I, S.D
    nps = L["nps"]
    with S.scope():
        wg = [S.sb("wg%d" % i, [128, KC, 512], BF16) for i in range(2)]
        wu = [S.sb("wu%d" % i, [128, KC, 512], BF16) for i in range(2)]
        wd = [S.sb("wd%d" % i, [128, 4, D], BF16) for i in range(2)]
        Xt = [S.sb("Xt%d" % i, [128, D], BF16) for i in range(2)]
        XTs = [S.sb("XT%d" % i, [128, KC, 512], BF16) for i in range(2)]

        def prep_x(e):
            for sbk in range(CAP // 128):
                x_ = Xt[sbk % 2]
                r0 = e * CAP + sbk * 128
                Dm("sp", out=x_.v, in_=L["Xs_d"].v[r0:r0 + 128, :])
                L["transpose_to"](lambda c: XTs[e % 2][:, c, sbk * 128:(sbk + 1) * 128], x_, 128, KC,
                                  evac=("act" if sbk % 2 else "dve"))
        sl = [S.sb("sl%d" % i, [128, 512], F32) for i in range(2)]
        aT = S.sb("aT", [128, 4, 512], BF16)
        Yt = [S.sb("Yt%d" % i, [128, D], F32) for i in range(2)]
        yi = 0
        for e in range(NEXP + 1):
            g_, u_, d_ = wg[e % 2], wu[e % 2], wd[e % 2]
            Dm("pool", out=g_.v, in_=L["w_eg"].v[e].rearrange("(kc p) n -> p kc n", p=128))
            Dm("pool", out=u_.v, in_=L["w_eu"].v[e].rearrange("(kc p) n -> p kc n", p=128))
            Dm("pool", out=d_.v, in_=L["w_ed"].v[e].rearrange("(kc p) n -> p kc n", p=128))
            tiles = [(0, CAP)] if e < NEXP else TT
            if e == 0:
                prep_x(0)
            for tix, (t0, n) in enumerate(tiles):
                if e < NEXP:
                    XT = XTs[e % 2]
                else:
                    XT = XTs[tix % 2]
                    Dm("sp", out=XT[:, :, :n], in_=L["h2T_d"].v[:, :, t0:t0 + n])
                for hc in range(4):
                    pg, pu = nps(), nps()
                    S.mm(pg[:, :n], [(g_[:, kc, hc * 128:(hc + 1) * 128], XT[:, kc, :n]) for kc in range(KC)])
                    S.mm(pu[:, :n], [(u_[:, kc, hc * 128:(hc + 1) * 128], XT[:, kc, :n]) for kc in range(KC)])
                    s_ = sl[hc % 2]
                    I_("act", "activation", out=s_[:, :n], in_=pg[:, :n], func=AF.Silu)
                    I_("dve", "tensor_tensor", out=aT[:, hc, :n], in0=s_[:, :n], in1=pu[:, :n], op=ALU.mult, disjoint=True)
                if e + 1 < NEXP:
                    prep_x(e + 1)
                for sbk in range((n + 127) // 128):
                    nr = min(128, n - sbk * 128)
                    y_ = Yt[yi % 2]
                    yi += 1
                    for cg in range(4):
                        ps = nps()
                        S.mm(ps[:nr, :], [(aT[:, hc, sbk * 128:sbk * 128 + nr], d_[:, hc, cg * 512:(cg + 1) * 512])
                                          for hc in range(4)])
                        if cg % 2:
                            I_("act", "copy", out=y_[:nr, cg * 512:(cg + 1) * 512], in_=ps[:nr, :], disjoint=True)
                        else:
                            I_("dve", "tensor_copy", out=y_[:nr, cg * 512:(cg + 1) * 512], in_=ps[:nr, :], disjoint=True)
                    if e < NEXP:
                        r0 = e * CAP + t0 + sbk * 128
                        Dm("sp", out=L["Ys_d"].v[r0:r0 + nr, :], in_=y_[:nr, :], disjoint=True)
                    else:
                        r0 = t0 + sbk * 128
                        Dm("sp", out=L["Ysh_d"].v[r0:r0 + nr, :], in_=y_[:nr, :], disjoint=True)


def combine_stage(nc, S, L):
    I_, Dm = S.I, S.D
    idx8, w8 = L["idx8"], L["w8"]
    with S.scope():
        G = [S.sb("G%d" % i, [128, D], F32) for i in range(8)]
        acc = [S.sb("cacc%d" % i, [128, D], F32) for i in range(2)]
        x1t = [S.sb("x1t%d" % i, [128, D], F32) for i in range(2)]
        g2 = [S.sb("g2_%d" % i, [128, D], F32) for i in range(2)]
        gtmp = S.sb("gtmp2", [128, D], F32)
        junk = S.sb("junk2", [128, D], BF16)
        ss = S.sb("ss2", [128, 1], F32)
        rs = S.sb("rs2", [128, 1], F32)
        L["load_bc"](gtmp, L["g_ffn_post"].v[0:1, :], 128)
        for j in range(8):
            I_("pool", "memset", ap=G[j].v.ap, extra_writes=[G[j]], constant=0.0)
        for s in range(2):
            nr = 128 if s == 0 else 64
            L["load_mod"](g2[s], 5, s == 1)
            I_("dve", "tensor_tensor", out=g2[s][:nr, :], in0=g2[s][:nr, :], in1=gtmp[:nr, :], op=ALU.mult)
        for ti, (r0, nr) in enumerate(RT):
            s = 1 if r0 >= T else 0
            a_, x_ = acc[ti % 2], x1t[ti % 2]
            Dm("sp", out=a_[:nr, :], in_=L["Ysh_d"].v[r0:r0 + nr, :])
            Dm("sp", out=x_[:nr, :], in_=L["x1_d"].v[r0:r0 + nr, :])
            for j in range(8):
                Dm("pool", indirect=True, out=G[j][:nr, :], out_offset=None, in_=L["Ys_d"].v,
                   in_offset=(idx8[:nr, ti, j:j + 1], 0), bounds_check=NEXP * CAP - 1, oob_is_err=False)
            for j in range(8):
                I_("dve", "scalar_tensor_tensor", out=a_[:nr, :], in0=G[j][:nr, :], scalar=w8[:nr, ti, j:j + 1],
                   in1=a_[:nr, :], op0=ALU.mult, op1=ALU.add)
            I_("act", "activation", out=junk[:nr, :], in_=a_[:nr, :], func=AF.Square, accum_out=ss[:nr, :])
            L["rstd_from_ss"](rs, ss, nr, D)
            I_("dve", "scalar_tensor_tensor", out=a_[:nr, :], in0=a_[:nr, :], scalar=rs[:nr, 0:1], in1=g2[s][:nr, :],
               op0=ALU.mult, op1=ALU.mult)
            I_("dve", "tensor_tensor", out=a_[:nr, :], in0=a_[:nr, :], in1=x_[:nr, :], op=ALU.add)
            Dm("sp", out=L["y_out"].v[r0:r0 + nr, :], in_=a_[:nr, :], disjoint=True)
```

```python
import contextlib
import numpy as np
import ml_dtypes
import concourse.bass as bass
import concourse.mybir as mybir
from concourse.bass_utils import run_bass_kernel_spmd

F32 = mybir.dt.float32
BF16 = mybir.dt.bfloat16
I32 = mybir.dt.int32
AF = mybir.ActivationFunctionType
ALU = mybir.AluOpType
AX = mybir.AxisListType

T = 2048
NS = 64
NT = T + NS
D = 2048
KC = 16
EPS = 1e-6
CAP = 512
NEXP = 64
NEG = -1.0e30
MNEG = -30000.0
NKS = 8320
BIS_ITERS = 17
STAGES = 99
DEBUG_NAMES = set()

TT = [(0, 512), (512, 512), (1024, 512), (1536, 512), (2048, 64)]
RT = [(i * 128, 128) for i in range(16)] + [(2048, 64)]


class View:
    def __init__(self, buf, ap):
        self.buf = buf
        self.ap = ap

    def __getitem__(self, idx):
        return View(self.buf, self.ap[idx])

    def rearrange(self, pat, **kw):
        return View(self.buf, self.ap.rearrange(pat, **kw))

    def bitcast(self, dt):
        return View(self.buf, self.ap.bitcast(dt))

    def bcast(self, n):
        return View(self.buf, self.ap.partition_broadcast(n))

    def to_broadcast(self, shape):
        return View(self.buf, self.ap.to_broadcast(shape))


class Buf:
    def __init__(self, name, t, space):
        self.name = name
        self.t = t
        self.space = space
        self.writes = {}
        self.reads = {}
        self.dsem = None

    def __getitem__(self, idx):
        return View(self, self.t[idx])

    @property
    def v(self):
        return View(self, self.t if self.space == "dram" else self.t[:])


class SemObj:
    def __init__(self, h):
        self.h = h
        self.cnt = 0


WRITE_KEYS = ("out", "accum_out", "out_max", "out_indices")


class Sched:
    def __init__(self, nc, ndsem=88):
        self.nc = nc
        self.eng = {"pe": nc.tensor, "act": nc.scalar, "dve": nc.vector, "pool": nc.gpsimd, "sp": nc.sync}
        self.esem = {k: SemObj(nc.alloc_semaphore("es_" + k)) for k in ("pe", "act", "dve", "pool")}
        self.esem_ids = {id(v) for v in self.esem.values()}
        self.arrive = nc.alloc_semaphore("bar_arrive")
        self.go = nc.alloc_semaphore("bar_go")
        self.epoch = 0
        self.free_dsems = [SemObj(nc.alloc_semaphore("ds%d" % i)) for i in range(ndsem)]
        self.all_dsems = list(self.free_dsems)
        self.seen = {k: {} for k in self.eng}
        self.scopes = []
        self.pe_pending = False
        self.bregs = {}
        self.uid = 0

    @contextlib.contextmanager
    def scope(self):
        st = contextlib.ExitStack()
        self.scopes.append((st, []))
        try:
            yield
        finally:
            self.barrier()
            st_, bufs = self.scopes.pop()
            for b in bufs:
                if b.dsem is not None:
                    self.free_dsems.append(b.dsem)
                    b.dsem = None
            st_.close()

    def sb(self, name, shape, dt):
        self.uid += 1
        nm = "%s_%d" % (name, self.uid)
        if self.scopes:
            t = self.scopes[-1][0].enter_context(self.nc.sbuf_tensor(nm, list(shape), dt))
        else:
            t = self.nc.alloc_sbuf_tensor(nm, list(shape), dt)
        b = Buf(nm, t, "sb")
        if self.scopes:
            self.scopes[-1][1].append(b)
        return b

    def ps(self, name, shape, dt=F32):
        t = self.nc.alloc_psum_tensor(name, list(shape), dt)
        return Buf(name, t, "ps")

    def dram(self, name, shape, dt, kind="Internal"):
        if kind == "Internal" and name in DEBUG_NAMES:
            kind = "ExternalOutput"
        t = self.nc.dram_tensor(name, list(shape), dt, kind=kind).ap()
        return Buf(name, t, "dram")

    def _dsem(self, b):
        if b.dsem is None:
            b.dsem = self.free_dsems.pop()
        return b.dsem

    def _wait(self, e, deps):
        best = {}
        for (so, val, ep) in deps:
            if ep != self.epoch:
                continue
            k = id(so)
            if k not in best or best[k][1] < val:
                best[k] = (so, val)
        for so, val in best.values():
            if id(so) not in self.esem_ids:
                val = max(val, so.cnt)
            elif e == "pe" and so is self.esem["pe"]:
                continue
            if self.seen[e].get(id(so), 0) >= val:
                continue
            self.eng[e].wait_ge(so.h, val)
            self.seen[e][id(so)] = val

    def _collect(self, kw):
        reads, writes = [], []
        for k, v in kw.items():
            if isinstance(v, View):
                (writes if k in WRITE_KEYS else reads).append(v.buf)
        return reads, writes

    def _record(self, tag, reads, writes, disjoint):
        k = id(tag[0])
        for w in writes:
            if not disjoint:
                w.writes = {}
                w.reads = {}
            if k not in w.writes or w.writes[k][2] != tag[2] or w.writes[k][1] < tag[1]:
                w.writes[k] = tag
        for r in reads:
            if k not in r.reads or r.reads[k][2] != tag[2] or r.reads[k][1] < tag[1]:
                r.reads[k] = tag

    def _deps(self, reads, writes, disjoint):
        deps = []
        for r in reads:
            deps += list(r.writes.values())
        for w in writes:
            deps += list(w.reads.values())
            if not disjoint:
                deps += list(w.writes.values())
        return deps

    def I(self, e, name, disjoint=False, inc=True, extra_reads=(), extra_writes=(), **kw):
        reads, writes = self._collect(kw)
        reads += list(extra_reads)
        writes += list(extra_writes)
        self._wait(e, self._deps(reads, writes, disjoint))
        args = {k: (v.ap if isinstance(v, View) else v) for k, v in kw.items()}
        ins = getattr(self.eng[e], name)(**args)
        so = self.esem[e]
        if inc:
            so.cnt += 1
            ins.then_inc(so.h, 1)
            tag = (so, so.cnt, self.epoch)
        else:
            assert e == "pe"
            tag = (so, so.cnt + 1, self.epoch)
        self._record(tag, reads, writes, disjoint)
        return ins

    def D(self, q, disjoint=False, semof=None, indirect=False, **kw):
        reads, writes = self._collect(kw)
        for k in ("out_offset", "in_offset"):
            if kw.get(k) is not None:
                reads.append(kw[k][0].buf)
        self._wait(q, self._deps(reads, writes, disjoint))
        if semof is None:
            cands = [b for b in writes + reads if b.space == "sb"]
            semof = cands[0] if cands else (writes + reads)[0]
        so = self._dsem(semof)
        args = {}
        for k, v in kw.items():
            if k in ("out_offset", "in_offset"):
                args[k] = None if v is None else bass.IndirectOffsetOnAxis(ap=v[0].ap, axis=v[1])
            else:
                args[k] = v.ap if isinstance(v, View) else v
        if indirect:
            bc = args.get("bounds_check")
            if isinstance(bc, int):
                if bc not in self.bregs:
                    r = self.nc.gpsimd.alloc_register("bnd%d" % bc)
                    self.nc.gpsimd.reg_mov(r, bc)
                    self.bregs[bc] = r
                args["bounds_check"] = self.bregs[bc]
            ins = self.eng[q].indirect_dma_start(**args)
        else:
            ins = self.eng[q].dma_start(**args)
        so.cnt += 16
        ins.then_inc(so.h, 16)
        self._record((so, so.cnt, self.epoch), reads, writes, disjoint)
        return ins

    def barrier(self):
        allsems = list(self.esem.values()) + [s for s in self.all_dsems if s.cnt > 0]
        for e in self.eng:
            for so in allsems:
                if self.seen[e].get(id(so), 0) < so.cnt:
                    self.eng[e].wait_ge(so.h, so.cnt)
                    self.seen[e][id(so)] = so.cnt

    def mm(self, out, pairs, last_inc=True):
        n = len(pairs)
        for i, (l, r) in enumerate(pairs):
            self.I("pe", "matmul", out=out, lhsT=l, rhs=r, start=(i == 0), stop=(i == n - 1),
                   inc=(i == n - 1))


def build(stages=STAGES):
    nc = bass.Bass("TRN2", target_bir_lowering=False)
    S = Sched(nc)
    I_, Dm = S.I, S.D

    def din(name, shape, dt=F32):
        return S.dram(name, shape, dt, "ExternalInput")

    def dout(name, shape, dt=F32):
        return S.dram(name, shape, dt, "ExternalOutput")

    xin = din("xin", [NT, D])
    cT = din("cT", [128, KC, 5])
    w_ada = din("w_ada", [D, 6 * D])
    b_ada = din("b_ada", [1, 6 * D])
    g_mix_pre = din("g_mix_pre", [1, D])
    g_mix_post = din("g_mix_post", [1, D])
    w_in = din("w_in", [D, 8784])
    w_oa = din("w_oa", [1024, D])
    wdwT = din("wdwT", [128, 8, 31])
    bdwT = din("bdwT", [128, 8])
    lngT = din("lngT", [128, 8])
    lnbT = din("lnbT", [128, 8])
    w_pw2 = din("w_pw2", [1024, D])
    w_o = din("w_o", [D, D])
    g_ffn_pre = din("g_ffn_pre", [1, D])
    g_ffn_post = din("g_ffn_post", [1, D])
    w_router = din("w_router", [D, NEXP])
    b_router = din("b_router", [1, NEXP])
    w_eg = din("w_eg", [NEXP + 1, D, 512])
    w_eu = din("w_eu", [NEXP + 1, D, 512])
    w_ed = din("w_ed", [NEXP + 1, 512, D])
    cache_k = din("cache_k", [2560, 128 * 256])
    cache_v = din("cache_v", [2560, 128 * 256])
    cache_ki = din("cache_ki", [2560, 128 * 64])
    state_conv = din("state_conv", [4, 30, 1024])
    ptT = din("ptT", [64, 4], I32)
    c_identb = din("c_identb", [128, 128], BF16)
    c_identf = din("c_identf", [128, 128])
    c_cbP = din("c_cbP", [128, 128])
    c_cbS = din("c_cbS", [16, 128])
    c_triu = din("c_triu", [128, 128], BF16)
    c_eoff = din("c_eoff", [128, NEXP])
    c_vmask = din("c_vmask", [64, 1])
    c_selB = din("c_selB", [4, 16])

    y_out = dout("y_out", [NT, D])
    k_out = dout("k_out", [NT, 256])
    v_out = dout("v_out", [NT, 256])
    ki_out = dout("ki_out", [NT, 64])
    convp_out = dout("convp_out", [30, 1024])
    convs_out = dout("convs_out", [4, 30, 1024])

    mod_d = S.dram("mod_d", [5, 6 * D], F32)
    hT_d = S.dram("hT_d", [128, KC, NT], BF16)
    qT_d = S.dram("qT_d", [128, 8, NT], BF16)
    qiT_d = S.dram("qiT_d", [128, 8, NT], BF16)
    wi_d = S.dram("wi_d", [NT, 16], F32)
    sT_d = S.dram("sT_d", [128, 8, NT], BF16)
    aoT_d = S.dram("aoT_d", [128, 8, NT], BF16)
    mT_d = S.dram("mT_d", [128, KC, NT], BF16)
    x1_d = S.dram("x1_d", [NT, D], F32)
    h2T_d = S.dram("h2T_d", [128, KC, NT], BF16)
    Xs_d = S.dram("Xs_d", [NEXP * CAP, D], BF16)
    Ys_d = S.dram("Ys_d", [NEXP * CAP, D], F32)
    Ysh_d = S.dram("Ysh_d", [NT, D], F32)
    Ks_d = S.dram("Ks_d", [4, NKS, 256], F32)
    Vs_d = S.dram("Vs_d", [4, NKS, 256], F32)
    kis_d = S.dram("kis_d", [4, NKS, 64], F32)

    PS = [S.ps("ps%d" % i, [128, 512], F32) for i in range(6)]
    PB = [S.ps("pb%d" % i, [128, 1024], BF16) for i in range(2)]
    psi = [0]

    def nps():
        psi[0] = (psi[0] + 1) % nrot[0]
        return PS[psi[0]]

    nrot = [4]

    PSO = PS[5]
    PSI = PS[4]

    pbi = [0]

    def npb():
        pbi[0] = (pbi[0] + 1) % len(PB)
        return PB[pbi[0]]

    identb = S.sb("identb", [128, 128], BF16)
    identf = S.sb("identf", [128, 128], F32)
    onesf = S.sb("onesf", [128, 128], F32)
    onesb = S.sb("onesb", [128, 128], BF16)
    Dm("sp", out=identb.v, in_=c_identb.v)
    Dm("sp", out=identf.v, in_=c_identf.v)
    I_("dve", "memset", ap=onesf.v.ap, extra_writes=[onesf], constant=1.0)
    I_("dve", "memset", ap=onesb.v.ap, extra_writes=[onesb], constant=1.0)
    idx8 = S.sb("idx8", [128, 17, 8], I32)
    w8 = S.sb("w8", [128, 17, 8], F32)

    def wview(w, c0, ncol, k0=0, kc=KC):
        return w.v[k0 * 128:(k0 + kc) * 128, c0:c0 + ncol].rearrange("(kc p) n -> p kc n", p=128)

    def load_bc(dst, src_row_view, nparts, p0=0):
        Dm("sp", out=dst[p0:p0 + nparts, :], in_=src_row_view.bcast(nparts), disjoint=(p0 != 0))

    def load_mod(dst, which, sample):
        c0 = which * D
        if not sample:
            load_bc(dst, mod_d.v[0:1, c0:c0 + D], 128)
        else:
            for b in range(4):
                load_bc(dst, mod_d.v[1 + b:2 + b, c0:c0 + D], 16, p0=16 * b)

    def rstd_from_ss(rstd, ss, nr, n):
        I_("dve", "tensor_scalar", out=rstd[:nr, :], in0=ss[:nr, :], scalar1=1.0 / n, scalar2=EPS,
           op0=ALU.mult, op1=ALU.add)
        I_("act", "sqrt", out=rstd[:nr, :], in_=rstd[:nr, :])
        I_("dve", "reciprocal", out=rstd[:nr, :], in_=rstd[:nr, :])

    def transpose_to(dst_fn, src, nr, nchunks, evac="act"):
        for c0 in range(0, nchunks, 4):
            pb = npb()
            for c in range(c0, min(c0 + 4, nchunks)):
                I_("pe", "transpose", out=pb[:, (c - c0) * 128:(c - c0) * 128 + nr],
                   in_=src[:nr, c * 128:(c + 1) * 128], identity=identb[:nr, :nr], disjoint=True)
            for c in range(c0, min(c0 + 4, nchunks)):
                srcv = pb[:, (c - c0) * 128:(c - c0) * 128 + nr]
                if evac == "act":
                    I_("act", "copy", out=dst_fn(c), in_=srcv, disjoint=True)
                else:
                    I_("dve", "tensor_copy", out=dst_fn(c), in_=srcv, disjoint=True)

    with S.scope():
        cTs = S.sb("cTs", [128, KC, 5], F32)
        Dm("sp", out=cTs.v, in_=cT.v)
        wb = [S.sb("wada%d" % i, [128, KC, 512], F32) for i in range(2)]
        bt = [S.sb("bt%d" % i, [5, 512], F32) for i in range(2)]
        mt = [S.sb("mt%d" % i, [5, 512], F32) for i in range(2)]
        for g in range(24):
            w = wb[g % 2]
            Dm("sp", out=w.v, in_=wview(w_ada, g * 512, 512))
            Dm("pool", out=bt[g % 2].v, in_=b_ada.v[0:1, g * 512:(g + 1) * 512].bcast(5))
            ps = nps()
            S.mm(ps[0:5, :], [(cTs[:, kc, :], w[:, kc, :]) for kc in range(KC)])
            I_("dve", "tensor_tensor", out=mt[g % 2].v, in0=ps[0:5, :], in1=bt[g % 2].v, op=ALU.add)
            Dm("pool", out=mod_d.v[:, g * 512:(g + 1) * 512], in_=mt[g % 2].v, disjoint=True)
    if stages <= 1:
        return finish(nc, S, [mod_d])

    with S.scope():
        hT = S.sb("hT", [128, KC, NT], BF16)
        with S.scope():
            gpre = S.sb("gpre", [128, D], F32)
            load_bc(gpre, g_mix_pre.v[0:1, :], 128)
            gs1 = [S.sb("gs1_%d" % i, [128, D], F32) for i in range(2)]
            sh1 = [S.sb("sh1_%d" % i, [128, D], F32) for i in range(2)]
            for s in range(2):
                load_mod(gs1[s], 1, s == 1)
                load_mod(sh1[s], 0, s == 1)
                nr = 128 if s == 0 else 64
                I_("dve", "scalar_tensor_tensor", out=gs1[s][:nr, :], in0=gs1[s][:nr, :], scalar=1.0,
                   in1=gpre[:nr, :], op0=ALU.add, op1=ALU.mult)
            xt = [S.sb("xt%d" % i, [128, D], F32) for i in range(2)]
            junk = S.sb("junk", [128, D], BF16)
            xh = S.sb("xh", [128, D], F32)
            xhb = [S.sb("xhb%d" % i, [128, D], BF16) for i in range(2)]
            ss = [S.sb("ss%d" % i, [128, 1], F32) for i in range(2)]
            rs = [S.sb("rs%d" % i, [128, 1], F32) for i in range(2)]
            for ti, (r0, nr) in enumerate(RT):
                s = 1 if r0 >= T else 0
                x_ = xt[ti % 2]
                Dm("sp", out=x_[:nr, :], in_=xin.v[r0:r0 + nr, :])
                I_("act", "activation", out=junk[:nr, :], in_=x_[:nr, :], func=AF.Square,
                   accum_out=ss[ti % 2][:nr, :])
                rstd_from_ss(rs[ti % 2], ss[ti % 2], nr, D)
                I_("dve", "scalar_tensor_tensor", out=xh[:nr, :], in0=x_[:nr, :], scalar=rs[ti % 2][:nr, 0:1],
                   in1=gs1[s][:nr, :], op0=ALU.mult, op1=ALU.mult)
                hb = xhb[ti % 2]
                I_("dve", "tensor_tensor", out=hb[:nr, :], in0=xh[:nr, :], in1=sh1[s][:nr, :], op=ALU.add)
                transpose_to(lambda c: hT[:, c, r0:r0 + nr], hb, nr, KC)
            Dm("sp", out=hT_d.v, in_=hT.v)

        with S.scope():
            wbuf = [S.sb("wbuf%d" % i, [128, KC, 512], BF16) for i in range(2)]
            qo = [S.sb("qo%d" % i, [128, 512], BF16) for i in range(2)]
            gthunks = gather_prep(nc, S, locals())
            gi = 0
            for (dst, col0) in ((qT_d, 0), (qiT_d, 1536)):
                for g in range(2):
                    w = wbuf[gi % 2]
                    gi += 1
                    Dm("pool", out=w.v, in_=wview(w_in, col0 + g * 512, 512))
                    if gi >= 2:
                        for _ in range(12):
                            if gthunks:
                                gthunks.pop(0)()
                    for (t0, nt) in TT:
                        for hh in range(4):
                            ps = nps()
                            S.mm(ps[:, :nt], [(w[:, kc, hh * 128:(hh + 1) * 128], hT[:, kc, t0:t0 + nt])
                                              for kc in range(KC)])
                            q_ = qo[(hh) % 2]
                            I_("act", "copy", out=q_[:, :nt], in_=ps[:, :nt])
                            Dm("sp", out=dst.v[:, g * 4 + hh, t0:t0 + nt], in_=q_[:, :nt], disjoint=True)
            w = wbuf[gi % 2]
            gi += 1
            Dm("pool", out=w.v, in_=wview(w_in, 1024, 512))
            while gthunks:
                gthunks.pop(0)()
            wsm = S.sb("wsm", [128, KC, 80], BF16)
            Dm("pool", out=wsm.v, in_=wview(w_in, 2560, 80))
            kvt = [S.sb("kvt%d" % i, [128, 512], F32) for i in range(2)]
            kwt = [S.sb("kwt%d" % i, [128, 80], F32) for i in range(2)]
            wit = [S.sb("wit%d" % i, [128, 16], F32) for i in range(2)]
            for ti, (r0, nr) in enumerate(RT):
                ps = nps()
                S.mm(ps[:nr, :], [(hT[:, kc, r0:r0 + nr], w[:, kc, :]) for kc in range(KC)])
                kv = kvt[ti % 2]
                I_("act", "copy", out=kv[:nr, :], in_=ps[:nr, :])
                Dm("sp", out=k_out.v[r0:r0 + nr, :], in_=kv[:nr, 0:256], disjoint=True)
                Dm("sp", out=v_out.v[r0:r0 + nr, :], in_=kv[:nr, 256:512], disjoint=True)
                ps = nps()
                S.mm(ps[:nr, :80], [(hT[:, kc, r0:r0 + nr], wsm[:, kc, :]) for kc in range(KC)])
                kw_ = kwt[ti % 2]
                I_("dve", "tensor_copy", out=kw_[:nr, :], in_=ps[:nr, :80])
                Dm("sp", out=ki_out.v[r0:r0 + nr, :], in_=kw_[:nr, 0:64], disjoint=True)
                I_("dve", "tensor_scalar", out=wit[ti % 2][:nr, :], in0=kw_[:nr, 64:80], scalar1=1.0 / 32.0,
                   scalar2=None, op0=ALU.mult)
                Dm("sp", out=wi_d.v[r0:r0 + nr, :], in_=wit[ti % 2][:nr, :], disjoint=True)
        if stages <= 2:
            S.barrier()
            return finish(nc, S, [k_out, v_out, ki_out, wi_d, qT_d, qiT_d])

        with S.scope():
            conv_stage(nc, S, locals())
    if stages <= 3:
        return finish(nc, S, [k_out, v_out, ki_out, convp_out, convs_out, sT_d])
    gather_stage(nc, S, locals())
    attn_stage(nc, S, locals())
    if stages <= 4:
        return finish(nc, S, [k_out, v_out, ki_out, convp_out, convs_out, aoT_d])
    merge_stage(nc, S, locals())
    if stages <= 5:
        return finish(nc, S, [k_out, v_out, ki_out, convp_out, convs_out, x1_d, h2T_d, Xs_d])
    expert_stage(nc, S, locals())
    combine_stage(nc, S, locals())
    return finish(nc, S, [y_out, k_out, v_out, ki_out, convp_out, convs_out])


def conv_stage(nc, S, L):
    I_, Dm = S.I, S.D
    hT, w_in, nps, npb = L["hT"], L["w_in"], L["nps"], L["npb"]
    identf, onesf, wview = L["identf"], L["onesf"], L["wview"]
    wdwT, bdwT, lngT, lnbT = L["wdwT"], L["bdwT"], L["lngT"], L["lnbT"]
    state_conv, convp_out, convs_out, sT_d = L["state_conv"], L["convp_out"], L["convs_out"], L["sT_d"]
    wdw = S.sb("wdw", [128, 8, 31], F32)
    bdw = S.sb("bdw", [128, 8], F32)
    lng = S.sb("lng", [128, 8], F32)
    lnb = S.sb("lnb", [128, 8], F32)
    for d_, s_ in ((wdw, wdwT), (bdw, bdwT), (lng, lngT), (lnb, lnbT)):
        Dm("sp", out=d_.v, in_=s_.v)
    stcs = [S.sb("stc%d" % i, [30, 4, 128], F32) for i in range(2)]
    Dm("sp", out=convs_out.v[:, 0:26, :], in_=state_conv.v[:, 4:30, :], disjoint=True, semof=convs_out)
    wa = [S.sb("wa%d" % i, [128, KC, 128], BF16) for i in range(2)]
    wb = [S.sb("wb%d" % i, [128, KC, 128], BF16) for i in range(2)]
    ue = [S.sb("ue%d" % i, [128, 30 + T], F32) for i in range(1)]
    ues = [S.sb("ues%d" % i, [128, 4, 34], F32) for i in range(2)]
    sg = [S.sb("sg%d" % i, [128, 512], F32) for i in range(2)]
    us = S.sb("us", [128, 64], F32)
    acc = [S.sb("acc%d" % i, [128, NT], F32) for i in range(2)]
    accs = [S.sb("accs%d" % i, [128, 4, 4], F32) for i in range(2)]
    ybf = S.sb("ybf", [128, 8, NT], BF16)
    ub = S.sb("ub", [128, 30 + T], BF16)
    dg = [S.sb("dg%d" % i, [128, 31, 128], BF16) for i in range(2)]
    st1 = S.sb("st1", [128, NT], F32)
    st2 = S.sb("st2", [128, NT], F32)
    sq = [S.sb("sq%d" % i, [128, 512], F32) for i in range(2)]
    cpo = [S.sb("cpo%d" % i, [30, 128], F32) for i in range(2)]
    cso = [S.sb("cso%d" % i, [4, 128], F32) for i in range(4)]
    I_("dve", "memset", ap=ybf.v.ap, extra_writes=[ybf], constant=0.0)
    for i in range(1):
        I_("dve", "memset", ap=ue[i][:, 0:30].ap, extra_writes=[ue[i]], constant=0.0)
    for cc in range(8):
        a_, b_ = wa[cc % 2], wb[cc % 2]
        Dm("pool", out=a_.v, in_=wview(w_in, 2640 + cc * 128, 128))
        Dm("pool", out=b_.v, in_=wview(w_in, 3664 + cc * 128, 128))
        u_, us_ = ue[0], ues[cc % 2]
        stc = stcs[cc % 2]
        Dm("sp", out=stc.v, in_=state_conv.v[:, :, cc * 128:(cc + 1) * 128].rearrange("b t c -> t b c"))
        for b in range(4):
            ps = nps()
            I_("pe", "transpose", out=ps[:, 0:30], in_=stc[:30, b, :], identity=identf[:30, :30])
            I_("act", "copy", out=us_[:, b, 0:30], in_=ps[:, 0:30], disjoint=True)
        for ti, (t0, nt) in enumerate(TT):
            psa, psb = nps(), nps()
            S.mm(psa[:, :nt], [(a_[:, kc, :], hT[:, kc, t0:t0 + nt]) for kc in range(KC)])
            S.mm(psb[:, :nt], [(b_[:, kc, :], hT[:, kc, t0:t0 + nt]) for kc in range(KC)])
            s_ = sg[ti % 2]
            I_("act", "activation", out=s_[:, :nt], in_=psb[:, :nt], func=AF.Sigmoid)
            if t0 < T:
                I_("dve", "tensor_tensor", out=u_[:, 30 + t0:30 + t0 + nt], in0=psa[:, :nt], in1=s_[:, :nt],
                   op=ALU.mult, disjoint=True)
            else:
                I_("dve", "tensor_tensor", out=us[:, :nt], in0=psa[:, :nt], in1=s_[:, :nt], op=ALU.mult)
                for b in range(4):
                    I_("dve", "tensor_copy", out=us_[:, b, 30:34], in_=us[:, b * 16:b * 16 + 4], disjoint=True)
        ps = nps()
        I_("pe", "transpose", out=ps[0:30, 0:128], in_=u_[:, T:T + 30], identity=identf.v)
        I_("act", "copy", out=cpo[cc % 2].v, in_=ps[0:30, 0:128])
        Dm("sp", out=convp_out.v[:, cc * 128:(cc + 1) * 128], in_=cpo[cc % 2].v, disjoint=True)
        for b in range(4):
            ps = nps()
            I_("pe", "transpose", out=ps[0:4, 0:128], in_=us_[:, b, 30:34], identity=identf.v)
            I_("act", "copy", out=cso[b].v, in_=ps[0:4, 0:128])
            Dm("sp", out=convs_out.v[b, 26:30, cc * 128:(cc + 1) * 128], in_=cso[b].v, disjoint=True)
        eng = "dve"
        a = acc[cc % 2]
        as_ = accs[cc % 2]
        dg_ = dg[cc % 2]
        for j in range(31):
            if j % 3 == 0:
                I_("pool", "tensor_scalar", out=dg_[:, j, :], in0=L["identb"].v, scalar1=wdw[:, cc, j:j + 1],
                   scalar2=None, op0=ALU.mult, disjoint=True)
            elif j % 3 == 1:
                I_("dve", "tensor_scalar", out=dg_[:, j, :], in0=L["identb"].v, scalar1=wdw[:, cc, j:j + 1],
                   scalar2=None, op0=ALU.mult, disjoint=True)
            else:
                I_("act", "activation", out=dg_[:, j, :], in_=L["identb"].v, func=AF.Copy, scale=wdw[:, cc, j:j + 1],
                   disjoint=True)
        I_("act", "copy", out=ub.v, in_=u_.v)
        for (t0, nt) in TT[:4]:
            ps = nps()
            S.mm(ps[:, :nt], [(dg_[:, j, :], ub[:, j + t0:j + t0 + nt]) for j in range(31)])
            I_("act", "activation", out=a[:, t0:t0 + nt], in_=ps[:, :nt], func=AF.Identity, bias=bdw[:, cc:cc + 1],
               disjoint=True)
        I_(eng, "tensor_scalar", out=as_.v, in0=us_[:, :, 0:4], scalar1=wdw[:, cc, 0:1], scalar2=bdw[:, cc:cc + 1],
           op0=ALU.mult, op1=ALU.add)
        for j in range(1, 31):
            I_(eng, "scalar_tensor_tensor", out=as_.v, in0=us_[:, :, j:j + 4], scalar=wdw[:, cc, j:j + 1], in1=as_.v,
               op0=ALU.mult, op1=ALU.add)
        I_("act", "copy", out=ybf[:, cc, 0:T], in_=a[:, 0:T], disjoint=True)
        for b in range(4):
            I_("act", "copy", out=ybf[:, cc, T + b * 16:T + b * 16 + 4], in_=as_[:, b, :], disjoint=True)
        for ti, (t0, nt) in enumerate(TT):
            if t0 < T:
                src = a[:, t0:t0 + nt]
            else:
                I_("dve", "tensor_copy", out=us[:, :nt], in_=ybf[:, cc, t0:t0 + nt])
                src = us[:, :nt]
            q_ = sq[ti % 2]
            I_("act", "activation", out=q_[:, :nt], in_=src, func=AF.Square)
            p1, p2 = nps(), nps()
            I_("pe", "matmul", out=p1[:, :nt], lhsT=onesf.v, rhs=src, start=True, stop=True)
            I_("pe", "matmul", out=p2[:, :nt], lhsT=onesf.v, rhs=q_[:, :nt], start=True, stop=True)
            if cc == 0:
                I_("dve", "tensor_copy", out=st1[:, t0:t0 + nt], in_=p1[:, :nt], disjoint=True)
                I_("dve", "tensor_copy", out=st2[:, t0:t0 + nt], in_=p2[:, :nt], disjoint=True)
            else:
                I_("dve", "tensor_tensor", out=st1[:, t0:t0 + nt], in0=st1[:, t0:t0 + nt], in1=p1[:, :nt], op=ALU.add)
                I_("dve", "tensor_tensor", out=st2[:, t0:t0 + nt], in0=st2[:, t0:t0 + nt], in1=p2[:, :nt], op=ALU.add)
    tmp = acc[0]
    I_("dve", "tensor_scalar", out=st1.v, in0=st1.v, scalar1=1.0 / 1024, scalar2=None, op0=ALU.mult)
    I_("dve", "tensor_tensor", out=tmp[:, 0:NT - 64], in0=st1[:, 0:NT - 64], in1=st1[:, 0:NT - 64], op=ALU.mult)
    I_("dve", "scalar_tensor_tensor", out=st2[:, 0:NT - 64], in0=st2[:, 0:NT - 64], scalar=1.0 / 1024,
       in1=tmp[:, 0:NT - 64], op0=ALU.mult, op1=ALU.subtract)
    I_("dve", "tensor_tensor", out=us.v, in0=st1[:, T:NT], in1=st1[:, T:NT], op=ALU.mult)
    I_("dve", "scalar_tensor_tensor", out=st2[:, T:NT], in0=st2[:, T:NT], scalar=1.0 / 1024,
       in1=us.v, op0=ALU.mult, op1=ALU.subtract)
    I_("dve", "tensor_scalar", out=st2.v, in0=st2.v, scalar1=EPS, scalar2=None, op0=ALU.add)
    I_("act", "sqrt", out=st2.v, in_=st2.v)
    I_("dve", "reciprocal", out=st2.v, in_=st2.v)
    tn = acc[1]
    so_ = [S.sb("so%d" % i, [128, NT], BF16) for i in range(1)]
    for cc in range(8):
        I_("dve", "tensor_tensor", out=tn.v, in0=ybf[:, cc, :], in1=st1.v, op=ALU.subtract)
        I_("dve", "tensor_tensor", out=tn.v, in0=tn.v, in1=st2.v, op=ALU.mult)
        o = so_[0]
        I_("act", "activation", out=o.v, in_=tn.v, func=AF.Silu, scale=lng[:, cc:cc + 1], bias=lnb[:, cc:cc + 1])
        Dm("sp", out=sT_d.v[:, cc, :], in_=o.v, disjoint=True)


def finish(nc, S, outs):
    S.barrier()
    return nc


def _consts():
    bf = ml_dtypes.bfloat16
    ident = np.eye(128, dtype=np.float32)
    r = np.arange(128)
    cbP = np.where(r[None, :] <= r[:, None], 0.0, NEG).astype(np.float32)
    cbS = np.full((16, 128), NEG, np.float32)
    for row in range(16):
        q = row % 4
        for qq in range(q + 1):
            cbS[row, qq] = 0.0
    triu = (r[:, None] < r[None, :]).astype(np.float32)
    eoff = np.tile((np.arange(NEXP) * CAP).astype(np.float32)[None, :], (128, 1))
    vmask = (np.arange(64) % 16 < 4).astype(np.float32)[:, None]
    return {"c_identb": ident.astype(bf), "c_identf": ident, "c_cbP": cbP, "c_cbS": cbS,
            "c_triu": triu.astype(bf), "c_eoff": eoff, "c_vmask": vmask,
            "c_selB": (np.arange(16)[None, :] % 4 == np.arange(4)[:, None]).astype(np.float32)}


def make_in_maps(inp, cores=range(8)):
    f = lambda a: np.ascontiguousarray(a)
    shared = {
        "w_ada": inp["w_ada"][0], "b_ada": inp["b_ada"][0][None, :],
        "g_mix_pre": inp["g_mix_pre"], "g_mix_post": inp["g_mix_post"],
        "w_in": inp["w_in"][0], "w_oa": inp["w_oa"][0],
        "wdwT": f(inp["w_dw"][0].T.reshape(8, 128, 31).transpose(1, 0, 2)),
        "bdwT": f(inp["b_dw"][0].reshape(8, 128).T), "lngT": f(inp["ln_conv_g"][0].reshape(8, 128).T),
        "lnbT": f(inp["ln_conv_b"][0].reshape(8, 128).T),
        "w_pw2": inp["w_pw2"][0], "w_o": inp["w_o"][0],
        "g_ffn_pre": inp["g_ffn_pre"], "g_ffn_post": inp["g_ffn_post"],
        "w_router": inp["w_router"][0], "b_router": inp["b_router"],
        "w_eg": np.concatenate([inp["w_exp_gate"][0], inp["w_sh_gate"]], 0),
        "w_eu": np.concatenate([inp["w_exp_up"][0], inp["w_sh_up"]], 0),
        "w_ed": np.concatenate([inp["w_exp_down"][0], inp["w_sh_down"]], 0),
        "cache_k": inp["cache_k"][0].reshape(2560, -1), "cache_v": inp["cache_v"][0].reshape(2560, -1),
        "cache_ki": inp["cache_kidx"][0].reshape(2560, -1),
    }
    shared.update(_consts())
    shared = {k: f(np.asarray(v)) for k, v in shared.items()}
    maps = []
    for c in cores:
        xs = inp["x_sample"][4 * c:4 * c + 4]
        xs_v = np.broadcast_to(xs[:, None, :, :], (4, 4, 4, D)).reshape(64, D)
        xin = np.concatenate([inp["x_prompt"][c], xs_v], 0)
        cc = np.concatenate([inp["c_prompt"][c:c + 1], inp["c_sample"][4 * c:4 * c + 4]], 0)
        cT = f(cc.T.reshape(KC, 128, 5).transpose(1, 0, 2))
        m = dict(shared)
        m.update({"xin": f(xin), "cT": cT, "state_conv": f(inp["state_conv"][0, 4 * c:4 * c + 4]),
                  "ptT": f(inp["page_table"][4 * c:4 * c + 4].T.astype(np.int32))})
        maps.append(m)
    return maps


_NC_CACHE = {}


def kernel(**inputs):
    inp = {k: np.asarray(v) for k, v in inputs.items()}
    if "nc" not in _NC_CACHE:
        _NC_CACHE["nc"] = build()
    nc = _NC_CACHE["nc"]
    maps = make_in_maps(inp)
    res = run_bass_kernel_spmd(nc, maps, core_ids=list(range(8)))
    R = res.results
    B = 8
    y_p = np.stack([R[c]["y_out"][:T] for c in range(B)])
    sel = lambda a: a[T:].reshape(4, 4, 4, -1)[:, 0]
    y_s = np.concatenate([sel(R[c]["y_out"]) for c in range(B)], 0)
    k_p = np.stack([R[c]["k_out"][:T] for c in range(B)]).reshape(1, B, T, 2, 128)
    v_p = np.stack([R[c]["v_out"][:T] for c in range(B)]).reshape(1, B, T, 2, 128)
    i_p = np.stack([R[c]["ki_out"][:T] for c in range(B)]).reshape(1, B, T, 64)
    c_p = np.stack([R[c]["convp_out"] for c in range(B)]).reshape(1, B, 30, 1024)
    k_s = np.concatenate([sel(R[c]["k_out"]) for c in range(B)], 0).reshape(1, 32, 4, 2, 128)
    v_s = np.concatenate([sel(R[c]["v_out"]) for c in range(B)], 0).reshape(1, 32, 4, 2, 128)
    i_s = np.concatenate([sel(R[c]["ki_out"]) for c in range(B)], 0).reshape(1, 32, 4, 64)
    c_s = np.concatenate([R[c]["convs_out"] for c in range(B)], 0).reshape(1, 32, 30, 1024)
    return tuple(np.ascontiguousarray(a, dtype=np.float32) for a in
                 (y_p, y_s, k_p, v_p, i_p, c_p, k_s, v_s, i_s, c_s))


def gather_prep(nc, S, L):
    I_, Dm = S.I, S.D
    ptb = S.sb("ptb", [64, 4], I32)
    Dm("sp", out=ptb.v, in_=L["ptT"].v)
    gb = [S.sb("gb%d" % i, [64, 8192], F32) for i in range(2)]
    ptf = S.sb("ptf", [64, 4], F32)
    I_("dve", "tensor_copy", out=ptf.v, in_=ptb.v)
    pcf = S.sb("pcf", [64, 4, 4], F32)
    pci = S.sb("pci", [64, 4, 4], I32)
    for ch in range(4):
        I_("dve", "tensor_scalar", out=pcf[:, :, ch], in0=ptf.v, scalar1=4.0, scalar2=float(ch),
           op0=ALU.mult, op1=ALU.add, disjoint=True)
    I_("dve", "tensor_copy", out=pci.v, in_=pcf.v)
    thunks = []
    gi = [0]
    for b in range(4):
        for (cache, dst, w) in ((L["cache_k"], L["Ks_d"], 256), (L["cache_v"], L["Vs_d"], 256),
                                (L["cache_ki"], L["kis_d"], 64)):
            spc = 8192 // w
            nch = 128 // spc
            for ch in range(nch):
                def th(b=b, cache=cache, dst=dst, w=w, spc=spc, nch=nch, ch=ch):
                    g = gb[gi[0] % 2]
                    gi[0] += 1
                    if nch == 1:
                        src, off, bc_ = cache.v, ptb[:, b:b + 1], 2559
                    else:
                        src, off, bc_ = cache.v.rearrange("p (c x) -> (p c) x", c=nch), pci[:, b, ch:ch + 1], 2560 * nch - 1
                    Dm("pool", indirect=True, out=g.v, out_offset=None, in_=src, in_offset=(off, 0),
                       bounds_check=bc_, oob_is_err=False)
                    dv = dst.v[b, 0:8192, :].rearrange("(j s) d -> j s d", s=128)[:, ch * spc:(ch + 1) * spc, :]
                    Dm("sp", out=dv, in_=g.v.rearrange("j (s d) -> j s d", d=w), disjoint=True)
                thunks.append(th)
    return thunks


def gather_stage(nc, S, L):
    I_, Dm = S.I, S.D
    with S.scope():
        zt = S.sb("zt", [112, 256], F32)
        I_("dve", "memset", ap=zt.v.ap, extra_writes=[zt], constant=0.0)
        for b in range(4):
            r0 = T + b * 16
            Dm("sp", out=L["Ks_d"].v[b, 8192:8208, :], in_=L["k_out"].v[r0:r0 + 16, :], disjoint=True, semof=L["Ks_d"])
            Dm("sp", out=L["Vs_d"].v[b, 8192:8208, :], in_=L["v_out"].v[r0:r0 + 16, :], disjoint=True, semof=L["Vs_d"])
            Dm("sp", out=L["kis_d"].v[b, 8192:8208, :], in_=L["ki_out"].v[r0:r0 + 16, :], disjoint=True, semof=L["kis_d"])
            Dm("sp", out=L["Ks_d"].v[b, 8208:NKS, :], in_=zt.v, disjoint=True)
            Dm("sp", out=L["Vs_d"].v[b, 8208:NKS, :], in_=zt.v, disjoint=True)
            Dm("sp", out=L["kis_d"].v[b, 8208:NKS, :], in_=zt[:, 0:64], disjoint=True)


def attn_stage(nc, S, L):
    I_, Dm = S.I, S.D
    nps, npb, PSO, PSI = L["nps"], L["npb"], L["PSO"], L["PSI"]
    identb, identf = L["identb"], L["identf"]
    SCALE = 128 ** -0.5
    with S.scope():
        KT = S.sb("KT", [128, 2, NKS], BF16)
        kiT2 = S.sb("kiT2", [128, NKS], BF16)
        V = S.sb("V", [128, NKS // 128, 256], BF16)
        Ib = S.sb("Ib", [128, NKS], F32)
        mb = S.sb("mb", [128, NKS], BF16)
        junkc = mb
        kin = [S.sb("kin%d" % i, [128, 4, 256], F32) for i in range(2)]
        vin = [S.sb("vin%d" % i, [128, 4, 256], F32) for i in range(2)]
        kiin = [S.sb("kiin%d" % i, [128, 4, 128], F32) for i in range(2)]
        wdg = [S.sb("wdg%d" % i, [128, 16, 128], BF16) for i in range(2)]
        qst = S.sb("qst", [128, 32], BF16)
        A3 = S.sb("A3", [4, 2, 8, 4], F32)
        selB = S.sb("selB", [4, 16], F32)
        Dm("sp", out=selB.v, in_=L["c_selB"].v)
        Wsel = [S.sb("Wsel%d" % i, [32, 16], BF16) for i in range(2)]
        rls = [S.sb("rls%d" % i, [32, 512], BF16) for i in range(4)]
        qTb = [S.sb("qTb%d" % i, [128, 8, 128], BF16) for i in range(2)]
        qiTb = [S.sb("qiTb%d" % i, [128, 8, 128], BF16) for i in range(2)]
        wib = [S.sb("wib%d" % i, [128, 16], F32) for i in range(2)]
        rl = [S.sb("rl%d" % i, [128, 512], BF16) for i in range(4)]
        Sm = [S.sb("Sm%d" % i, [128, 512], F32) for i in range(2)]
        P = [S.sb("P%d" % i, [128, 512], BF16) for i in range(2)]
        PT = [S.sb("PT%d" % i, [128, 4, 128], BF16) for i in range(2)]
        rsum = S.sb("rsum", [128, 20], F32)
        sm = {k: S.sb("sm_" + k, [128, 1], F32) for k in ("lo", "hi", "w0", "mid", "cnt", "step", "thr", "rs", "rinv")}
        Osb = S.sb("Osb", [128, 128], BF16)
        aoTb = [S.sb("aoTb%d" % i, [128, 8, 128], BF16) for i in range(2)]
        aoTs = S.sb("aoTs", [128, 8, 64], BF16)
        qvs = [S.sb("qvs%d" % i, [128, 16], BF16) for i in range(2)]
        cbP = S.sb("cbP", [128, 128], F32)
        cbS = S.sb("cbS", [16, 128], F32)
        Dm("sp", out=cbP.v, in_=L["c_cbP"].v)
        Dm("sp", out=cbS.v, in_=L["c_cbS"].v)
        I_("dve", "memset", ap=aoTs.v.ap, extra_writes=[aoTs], constant=0.0)

        def load_seq(Ksrc, Vsrc, kisrc, nblk):
            for g0 in range(0, nblk, 4):
                ng = min(4, nblk - g0)
                k_, v_, i_ = kin[(g0 // 4) % 2], vin[(g0 // 4) % 2], kiin[(g0 // 4) % 2]
                rows = slice(g0 * 128, (g0 + ng) * 128)
                Dm("sp", out=k_[:, 0:ng, :], in_=Ksrc[rows, :].rearrange("(j p) d -> p j d", p=128))
                Dm("sp", out=v_[:, 0:ng, :], in_=Vsrc[rows, :].rearrange("(j p) d -> p j d", p=128))
                Dm("sp", out=i_[:, 0:ng, 0:64], in_=kisrc[rows, :].rearrange("(j p) d -> p j d", p=128))
                Dm("sp", out=i_[:, 0:ng, 64:128], in_=kisrc[rows, :].rearrange("(j p) d -> p j d", p=128), disjoint=True)
                I_("pool", "tensor_copy", out=V[:, g0:g0 + ng, :], in_=v_[:, 0:ng, :], disjoint=True)
                for kvh in range(2):
                    ps = nps()
                    for j in range(ng):
                        I_("pe", "transpose", out=ps[:, j * 128:(j + 1) * 128], in_=k_[:, j, kvh * 128:(kvh + 1) * 128],
                           identity=identf.v, disjoint=True)
                    I_("act", "copy", out=KT[:, kvh, g0 * 128:(g0 + ng) * 128], in_=ps[:, 0:ng * 128], disjoint=True)
                ps = nps()
                for j in range(ng):
                    I_("pe", "transpose", out=ps[:, j * 128:(j + 1) * 128], in_=i_[:, j, :], identity=identf.v, disjoint=True)
                I_("dve", "tensor_copy", out=kiT2[:, g0 * 128:(g0 + ng) * 128], in_=ps[:, 0:ng * 128], disjoint=True)

        cnt_ = [0]
        L["nrot"][0] = 3
        PSI2 = [PSI, L["PS"][3]]
        IbP = [Buf("IbP%d" % i, Ib.t[:, i * 2048:(i + 1) * 2048], "sb") for i in range(2)]
        mbP = [Buf("mbP%d" % i, mb.t[:, i * 2048:(i + 1) * 2048], "sb") for i in range(2)]

        def index(R, nk, qiTv, wiv, IbX, stacked=False):
            nkt = (nk + 511) // 512
            pi = [0]
            if not stacked:
                wd_ = wdg[cnt_[0] % 2]
                cnt_[0] += 1
                for h in range(16):
                    I_("pool", "tensor_scalar", out=wd_[:R, h, :R], in0=identb[:R, :R], scalar1=wiv[:R, h:h + 1],
                       scalar2=None, op0=ALU.mult, disjoint=True)
                ri = [0]

                def dots(kt, h):
                    k0 = kt * 512
                    kn = min(512, nk - k0)
                    hp, half = h // 2, h % 2
                    ps = nps()
                    I_("pe", "matmul", out=ps[:R, :kn], lhsT=qiTv[half * 64:(half + 1) * 64, hp, :R],
                       rhs=kiT2[half * 64:(half + 1) * 64, k0:k0 + kn], start=True, stop=True)
                    r_ = rl[ri[0] % 4]
                    ri[0] += 1
                    I_("act", "activation", out=r_[:R, :kn], in_=ps[:R, :kn], func=AF.Relu)
                    return r_

                def acc(kt, h, r_):
                    k0 = kt * 512
                    kn = min(512, nk - k0)
                    pI = PSI2[kt % 2]
                    I_("pe", "matmul", out=pI[:R, :kn], lhsT=wd_[:R, h, :R], rhs=r_[:R, :kn], start=(h == 0),
                       stop=(h == 15), inc=(h == 15))
                    if h == 15:
                        I_("dve", "tensor_copy", out=IbX[:R, k0:k0 + kn], in_=pI[:R, :kn], disjoint=True)

                pend = None
                for kt in range(nkt):
                    for h in range(16):
                        r_ = dots(kt, h)
                        if pend is not None:
                            acc(*pend)
                        pend = (kt, h, r_)
                acc(*pend)
            else:
                I_("dve", "tensor_copy", out=qst.v.rearrange("p (h q) -> p h q", q=4), in_=qiTv[:, :, 0:4])
                for half in range(2):
                    for qq in range(4):
                        I_("dve", "tensor_scalar", out=A3[:, half, :, qq], in0=wiv[0:4, half:16:2],
                           scalar1=identf[0:4, qq:qq + 1], scalar2=None, op0=ALU.mult, disjoint=True)
                    ps = nps()
                    I_("pe", "matmul", out=ps[0:32, 0:16], lhsT=A3[:, half, :, :].rearrange("p h q -> p (h q)"),
                       rhs=selB.v, start=True, stop=True)
                    I_("dve", "tensor_copy", out=Wsel[half].v, in_=ps[0:32, 0:16])
                ri = 0
                pend = None

                def sacc(kt, rr):
                    k0 = kt * 512
                    kn = min(512, nk - k0)
                    pI = PSI2[kt % 2]
                    for half in range(2):
                        I_("pe", "matmul", out=pI[:R, :kn], lhsT=Wsel[half].v, rhs=rr[half][:, :kn], start=(half == 0),
                           stop=(half == 1), inc=(half == 1))
                    I_("dve", "tensor_copy", out=IbX[:R, k0:k0 + kn], in_=pI[:R, :kn], disjoint=True)

                for kt in range(nkt):
                    k0 = kt * 512
                    kn = min(512, nk - k0)
                    rr = []
                    for half in range(2):
                        ps = nps()
                        I_("pe", "matmul", out=ps[0:32, :kn], lhsT=qst[half * 64:(half + 1) * 64, :],
                           rhs=kiT2[half * 64:(half + 1) * 64, k0:k0 + kn], start=True, stop=True)
                        r_ = rls[ri % 4]
                        ri += 1
                        I_("act", "activation", out=r_[:, :kn], in_=ps[0:32, :kn], func=AF.Relu)
                        rr.append(r_)
                    if pend is not None:
                        sacc(*pend)
                    pend = (kt, rr)
                sacc(*pend)

        def thresh(R, nk, IbX, mbX, cb, bisect):
            lo, hi, w0, mid, cnt, step, thr = (sm[k] for k in ("lo", "hi", "w0", "mid", "cnt", "step", "thr"))
            if bisect:
                I_("dve", "tensor_reduce", out=hi[:R, :], in_=IbX[:R, :nk], axis=AX.X, op=ALU.max)
                I_("dve", "tensor_reduce", out=lo[:R, :], in_=IbX[:R, :nk], axis=AX.X, op=ALU.min)
                I_("dve", "tensor_tensor", out=w0[:R, :], in0=hi[:R, :], in1=lo[:R, :], op=ALU.subtract)
            I_("dve", "tensor_tensor", out=IbX[:R, nk - 128:nk], in0=IbX[:R, nk - 128:nk], in1=cb[:R, :], op=ALU.add)
            if bisect:
                for it in range(BIS_ITERS):
                    f = 2.0 ** -(it + 1)
                    I_("dve", "scalar_tensor_tensor", out=mid[:R, :], in0=w0[:R, :], scalar=f, in1=lo[:R, :],
                       op0=ALU.mult, op1=ALU.add)
                    I_("dve", "tensor_scalar", out=mbX[:R, :nk], in0=IbX[:R, :nk], scalar1=mid[:R, 0:1], scalar2=0.0,
                       op0=ALU.is_ge, op1=ALU.add, accum_out=cnt[:R, :])
                    I_("dve", "tensor_scalar", out=step[:R, :], in0=cnt[:R, :], scalar1=255.5, scalar2=w0[:R, 0:1],
                       op0=ALU.is_ge, op1=ALU.mult)
                    I_("dve", "scalar_tensor_tensor", out=lo[:R, :], in0=step[:R, :], scalar=f, in1=lo[:R, :],
                       op0=ALU.mult, op1=ALU.add)
                thr_v = lo
            else:
                I_("dve", "memset", ap=thr[:R, :].ap, extra_writes=[thr], constant=-1.0e29)
                thr_v = thr
            I_("dve", "tensor_scalar", out=mbX[:R, :nk], in0=IbX[:R, :nk], scalar1=thr_v[:R, 0:1], scalar2=MNEG,
               op0=ALU.is_lt, op1=ALU.mult)

        def run_passes(R, nk, mbX, passes):
            nkt = (nk + 511) // 512
            nblk = nk // 128
            for (qv, kvh, out_fn) in passes:
                bi = [0]

                def emit_S(kt):
                    k0 = kt * 512
                    kn = min(512, nk - k0)
                    ps = nps()
                    I_("pe", "matmul", out=ps[:R, :kn], lhsT=qv, rhs=KT[:, kvh, k0:k0 + kn], start=True, stop=True)
                    s_, p_ = Sm[kt % 2], P[kt % 2]
                    I_("dve", "scalar_tensor_tensor", out=s_[:R, :kn], in0=ps[:R, :kn], scalar=SCALE,
                       in1=mbX[:R, k0:k0 + kn], op0=ALU.mult, op1=ALU.add)
                    I_("act", "activation", out=p_[:R, :kn], in_=s_[:R, :kn], func=AF.Exp,
                       accum_out=rsum[:R, kt:kt + 1], disjoint=True)
                    return p_

                def emit_PV(kt, p_):
                    k0 = kt * 512
                    kn = min(512, nk - k0)
                    nj = kn // 128
                    t_ = PT[kt % 2]
                    pb = npb()
                    for j in range(nj):
                        I_("pe", "transpose", out=pb[:, j * 128:j * 128 + R], in_=p_[:R, j * 128:(j + 1) * 128],
                           identity=identb[:R, :R], disjoint=True)
                    I_("act", "copy", out=t_[:, 0:nj, :R],
                       in_=pb[:, 0:nj * 128].rearrange("p (j r) -> p j r", r=128)[:, :, :R])
                    for j in range(nj):
                        I_("pe", "matmul", out=PSO[:R, 0:128], lhsT=t_[:, j, :R],
                           rhs=V[:, kt * 4 + j, kvh * 128:(kvh + 1) * 128], start=(bi[0] == 0),
                           stop=(bi[0] == nblk - 1), inc=(bi[0] == nblk - 1))
                        bi[0] += 1

                pend = None
                for kt in range(nkt):
                    p_ = emit_S(kt)
                    if pend is not None:
                        emit_PV(*pend)
                    pend = (kt, p_)
                emit_PV(*pend)
                I_("dve", "tensor_reduce", out=sm["rs"][:R, :], in_=rsum[:R, :nkt], axis=AX.X, op=ALU.add)
                I_("dve", "reciprocal", out=sm["rinv"][:R, :], in_=sm["rs"][:R, :])
                I_("act", "activation", out=Osb[:R, :], in_=PSO[:R, 0:128], func=AF.Copy, scale=sm["rinv"][:R, 0:1])
                pb = npb()
                I_("pe", "transpose", out=pb[:, 0:R], in_=Osb[:R, :], identity=identb[:R, :R])
                out_fn(pb)

        load_seq(L["k_out"].v, L["v_out"].v, L["ki_out"].v, 16)

        def qload(j):
            q0 = j * 128
            Dm("sp", out=qTb[j % 2].v, in_=L["qT_d"].v[:, :, q0:q0 + 128])
            Dm("sp", out=qiTb[j % 2].v, in_=L["qiT_d"].v[:, :, q0:q0 + 128])
            Dm("sp", out=wib[j % 2].v, in_=L["wi_d"].v[q0:q0 + 128, :])

        qload(0)
        index(128, 128, qiTb[0], wib[0], IbP[0])
        for j in range(16):
            q0 = j * 128
            qT_, ao_ = qTb[j % 2], aoTb[j % 2]
            thresh(128, (j + 1) * 128, IbP[j % 2], mbP[j % 2], cbP, bisect=(j >= 2))
            if j + 1 < 16:
                qload(j + 1)
                index(128, (j + 2) * 128, qiTb[(j + 1) % 2], wib[(j + 1) % 2], IbP[(j + 1) % 2])
            passes = []
            for h in range(8):
                def ofn(pb, h=h, ao_=ao_):
                    I_("dve", "tensor_copy", out=ao_[:, h, :], in_=pb[:, 0:128], disjoint=True)
                passes.append((qT_[:, h, :], h // 4, ofn))
            run_passes(128, (j + 1) * 128, mbP[j % 2], passes)
            Dm("sp", out=L["aoT_d"].v[:, :, q0:q0 + 128], in_=ao_.v, disjoint=True)
        S.barrier()
        for b in range(4):
            load_seq(L["Ks_d"].v[b], L["Vs_d"].v[b], L["kis_d"].v[b], NKS // 128)
            c0 = T + b * 16
            qT_, qiT_, wi_ = qTb[b % 2], qiTb[b % 2], wib[b % 2]
            Dm("sp", out=qT_[:, :, 0:16], in_=L["qT_d"].v[:, :, c0:c0 + 16])
            Dm("sp", out=qiT_[:, :, 0:16], in_=L["qiT_d"].v[:, :, c0:c0 + 16])
            Dm("sp", out=wi_[0:16, :], in_=L["wi_d"].v[c0:c0 + 16, :])
            passes = []
            for kvh in range(2):
                qv = qvs[kvh]
                for h4 in range(4):
                    I_("dve", "tensor_copy", out=qv[:, h4 * 4:h4 * 4 + 4], in_=qT_[:, kvh * 4 + h4, h4 * 4:h4 * 4 + 4],
                       disjoint=True)

                def ofn(pb, kvh=kvh, b=b):
                    for h4 in range(4):
                        I_("dve", "tensor_copy", out=aoTs[:, kvh * 4 + h4, b * 16:b * 16 + 4],
                           in_=pb[:, h4 * 4:h4 * 4 + 4], disjoint=True)
                passes.append((qv.v, kvh, ofn))
            index(16, NKS, qiT_, wi_, Ib, stacked=True)
            thresh(16, NKS, Ib, mb, cbS, True)
            run_passes(16, NKS, mb, passes)
        Dm("sp", out=L["aoT_d"].v[:, :, T:NT], in_=aoTs.v, disjoint=True)
        L["nrot"][0] = 4


def merge_stage(nc, S, L):
    I_, Dm = S.I, S.D
    nps, npb, wview = L["nps"], L["npb"], L["wview"]
    w_in = L["w_in"]
    with S.scope():
        hTt = S.sb("hTt", [128, KC, 512], BF16)
        aoTt = S.sb("aoTt", [128, 8, 512], BF16)
        sTt = S.sb("sTt", [128, 8, 512], BF16)
        mTt = S.sb("mTt", [128, KC, 512], BF16)
        wga = [S.sb("wga%d" % i, [128, KC, 128], BF16) for i in range(2)]
        wgb = [S.sb("wgb%d" % i, [128, KC, 128], BF16) for i in range(2)]
        woa = [S.sb("woa%d" % i, [128, 8, 128], BF16) for i in range(2)]
        wpw = [S.sb("wpw%d" % i, [128, 8, 128], BF16) for i in range(2)]
        sga = [S.sb("sga%d" % i, [128, 512], F32) for i in range(2)]
        sgb = [S.sb("sgb%d" % i, [128, 512], F32) for i in range(2)]
        wi_ = 0

        def load_w(i):
            fc = i % 16
            Dm("pool", out=wga[i % 2].v, in_=wview(w_in, 4688 + fc * 128, 128))
            Dm("pool", out=wgb[i % 2].v, in_=wview(w_in, 6736 + fc * 128, 128))
            Dm("pool", out=woa[i % 2].v, in_=wview(L["w_oa"], fc * 128, 128, kc=8))
            Dm("pool", out=wpw[i % 2].v, in_=wview(L["w_pw2"], fc * 128, 128, kc=8))

        load_w(0)
        for (t0, nt) in TT:
            Dm("sp", out=hTt[:, :, :nt], in_=L["hT_d"].v[:, :, t0:t0 + nt])
            Dm("sp", out=aoTt[:, :, :nt], in_=L["aoT_d"].v[:, :, t0:t0 + nt])
            Dm("sp", out=sTt[:, :, :nt], in_=L["sT_d"].v[:, :, t0:t0 + nt])
            for fc in range(16):
                a, b_, o, p = wga[wi_ % 2], wgb[wi_ % 2], woa[wi_ % 2], wpw[wi_ % 2]
                sa, sb_ = sga[wi_ % 2], sgb[wi_ % 2]
                wi_ += 1
                if wi_ < 16 * len(TT):
                    load_w(wi_)
                pga, pA, pgb, pB = nps(), nps(), nps(), nps()
                S.mm(pga[:, :nt], [(a[:, kc, :], hTt[:, kc, :nt]) for kc in range(KC)])
                S.mm(pA[:, :nt], [(o[:, kc, :], aoTt[:, kc, :nt]) for kc in range(8)])
                S.mm(pgb[:, :nt], [(b_[:, kc, :], hTt[:, kc, :nt]) for kc in range(KC)])
                S.mm(pB[:, :nt], [(p[:, kc, :], sTt[:, kc, :nt]) for kc in range(8)])
                I_("act", "activation", out=sa[:, :nt], in_=pga[:, :nt], func=AF.Sigmoid)
                I_("dve", "tensor_tensor", out=sa[:, :nt], in0=sa[:, :nt], in1=pA[:, :nt], op=ALU.mult)
                I_("act", "activation", out=sb_[:, :nt], in_=pgb[:, :nt], func=AF.Sigmoid)
                I_("dve", "tensor_tensor", out=sb_[:, :nt], in0=sb_[:, :nt], in1=pB[:, :nt], op=ALU.mult)
                I_("dve", "tensor_tensor", out=mTt[:, fc, :nt], in0=sa[:, :nt], in1=sb_[:, :nt], op=ALU.add, disjoint=True)
            Dm("sp", out=L["mT_d"].v[:, :, t0:t0 + nt], in_=mTt[:, :, :nt], disjoint=True)

    with S.scope():
        wo = S.sb("wo", [128, KC, D], BF16)
        for g in range(4):
            Dm("pool", out=wo[:, :, g * 512:(g + 1) * 512], in_=wview(L["w_o"], g * 512, 512), disjoint=True)
        wr = S.sb("wr", [128, KC, NEXP], BF16)
        Dm("pool", out=wr.v, in_=wview(L["w_router"], 0, NEXP))
        brt = S.sb("brt", [128, NEXP], F32)
        L["load_bc"](brt, L["b_router"].v[0:1, :], 128)
        triu = S.sb("triu", [128, 128], BF16)
        Dm("sp", out=triu.v, in_=L["c_triu"].v)
        eoff = S.sb("eoff", [128, NEXP], F32)
        Dm("sp", out=eoff.v, in_=L["c_eoff"].v)
        vmask = S.sb("vmask", [64, 1], F32)
        Dm("sp", out=vmask.v, in_=L["c_vmask"].v)
        carry = S.sb("carry", [128, NEXP], F32)
        I_("dve", "memset", ap=carry.v.ap, extra_writes=[carry], constant=0.0)
        osb = S.sb("osb", [128, D], F32)
        gtmp = osb
        g1 = [S.sb("g1_%d" % i, [128, D], F32) for i in range(2)]
        gs2 = [S.sb("gs2_%d" % i, [128, D], F32) for i in range(2)]
        sh2 = [S.sb("sh2_%d" % i, [128, D], F32) for i in range(2)]
        for s in range(2):
            nr = 128 if s == 0 else 64
            L["load_mod"](g1[s], 2, s == 1)
            L["load_bc"](gtmp, L["g_mix_post"].v[0:1, :], 128)
            I_("dve", "tensor_tensor", out=g1[s][:nr, :], in0=g1[s][:nr, :], in1=gtmp[:nr, :], op=ALU.mult)
            L["load_mod"](gs2[s], 4, s == 1)
            L["load_bc"](gtmp, L["g_ffn_pre"].v[0:1, :], 128)
            I_("dve", "scalar_tensor_tensor", out=gs2[s][:nr, :], in0=gs2[s][:nr, :], scalar=1.0, in1=gtmp[:nr, :],
               op0=ALU.add, op1=ALU.mult)
            L["load_mod"](sh2[s], 3, s == 1)
        mTr = [S.sb("mTr%d" % i, [128, KC, 128], BF16) for i in range(2)]
        xt = [S.sb("xt%d" % i, [128, D], F32) for i in range(1)]
        x1 = [S.sb("x1_%d" % i, [128, D], F32) for i in range(1)]
        junk = S.sb("junk", [128, D], BF16)
        h2b = [S.sb("h2b%d" % i, [128, D], BF16) for i in range(2)]
        h2Tt = [S.sb("h2Tt%d" % i, [128, KC, 128], BF16) for i in range(2)]
        ss = S.sb("ss", [128, 1], F32)
        rs = S.sb("rs", [128, 1], F32)
        sc = S.sb("sc", [128, NEXP], F32)
        sel = S.sb("sel", [128, NEXP], F32)
        selm = S.sb("selm", [128, NEXP], F32)
        g8 = S.sb("g8", [128, 8, 8], F32)
        gsc = S.sb("gsc", [128, 8], F32)
        m8 = S.sb("m8", [128, 8], F32)
        pen = S.sb("pen", [128, 8], F32)
        e8 = S.sb("e8", [128, 8], F32)
        em = S.sb("em", [128, NEXP], F32)
        emb = S.sb("emb", [128, NEXP], BF16)
        gw = S.sb("gw", [128, NEXP], F32)
        den = S.sb("den", [128, 1], F32)
        pos = S.sb("pos", [128, NEXP], F32)
        ok = S.sb("ok", [128, NEXP], F32)
        key = S.sb("key", [128, NEXP], F32)
        d8 = S.sb("d8", [128, 8], F32)
        neg = S.sb("neg", [128, 8], F32)
        oh = S.sb("oh", [128, NEXP], F32)
        idx8, w8 = L["idx8"], L["w8"]
        for ti, (r0, nr) in enumerate(RT):
            s = 1 if r0 >= T else 0
            m_, x_, x1_, hb, hT_ = mTr[ti % 2], xt[0], x1[0], h2b[ti % 2], h2Tt[ti % 2]
            Dm("sp", out=m_[:, :, :nr], in_=L["mT_d"].v[:, :, r0:r0 + nr])
            Dm("sp", out=x_[:nr, :], in_=L["xin"].v[r0:r0 + nr, :])
            for cg in range(4):
                ps = nps()
                S.mm(ps[:nr, :], [(m_[:, kc, :nr], wo[:, kc, cg * 512:(cg + 1) * 512]) for kc in range(KC)])
                I_("act", "copy", out=osb[:nr, cg * 512:(cg + 1) * 512], in_=ps[:nr, :], disjoint=True)
            I_("act", "activation", out=junk[:nr, :], in_=osb[:nr, :], func=AF.Square, accum_out=ss[:nr, :])
            L["rstd_from_ss"](rs, ss, nr, D)
            I_("dve", "scalar_tensor_tensor", out=osb[:nr, :], in0=osb[:nr, :], scalar=rs[:nr, 0:1], in1=g1[s][:nr, :],
               op0=ALU.mult, op1=ALU.mult)
            I_("dve", "tensor_tensor", out=x1_[:nr, :], in0=osb[:nr, :], in1=x_[:nr, :], op=ALU.add)
            Dm("sp", out=L["x1_d"].v[r0:r0 + nr, :], in_=x1_[:nr, :], disjoint=True)
            I_("act", "activation", out=junk[:nr, :], in_=x1_[:nr, :], func=AF.Square, accum_out=ss[:nr, :])
            L["rstd_from_ss"](rs, ss, nr, D)
            I_("dve", "scalar_tensor_tensor", out=osb[:nr, :], in0=x1_[:nr, :], scalar=rs[:nr, 0:1], in1=gs2[s][:nr, :],
               op0=ALU.mult, op1=ALU.mult)
            I_("dve", "tensor_tensor", out=hb[:nr, :], in0=osb[:nr, :], in1=sh2[s][:nr, :], op=ALU.add)
            L["transpose_to"](lambda c: hT_[:, c, :nr], hb, nr, KC)
            Dm("sp", out=L["h2T_d"].v[:, :, r0:r0 + nr], in_=hT_[:, :, :nr], disjoint=True)
            ps = nps()
            S.mm(ps[:nr, :NEXP], [(hT_[:, kc, :nr], wr[:, kc, :]) for kc in range(KC)])
            I_("act", "activation", out=sc[:nr, :], in_=ps[:nr, :NEXP], func=AF.Sigmoid)
            I_("dve", "tensor_tensor", out=sel[:nr, :], in0=sc[:nr, :], in1=brt[:nr, :], op=ALU.add)
            for g in range(8):
                I_("dve", "max", out=g8[:nr, g, :], in_=sel[:nr, g * 8:(g + 1) * 8], disjoint=True)
            I_("dve", "tensor_tensor", out=gsc[:nr, :], in0=g8[:nr, :, 0], in1=g8[:nr, :, 1], op=ALU.add)
            I_("dve", "max", out=m8[:nr, :], in_=gsc[:nr, :])
            I_("dve", "tensor_scalar", out=pen[:nr, :], in0=gsc[:nr, :], scalar1=m8[:nr, 3:4], scalar2=1.0e9,
               op0=ALU.is_lt, op1=ALU.mult)
            for g in range(8):
                I_("dve", "tensor_scalar", out=selm[:nr, g * 8:(g + 1) * 8], in0=sel[:nr, g * 8:(g + 1) * 8],
                   scalar1=pen[:nr, g:g + 1], scalar2=None, op0=ALU.subtract, disjoint=True)
            I_("dve", "max", out=e8[:nr, :], in_=selm[:nr, :])
            I_("dve", "tensor_scalar", out=em[:nr, :], in0=selm[:nr, :], scalar1=e8[:nr, 7:8], scalar2=None, op0=ALU.is_ge)
            I_("dve", "tensor_tensor", out=gw[:nr, :], in0=sc[:nr, :], in1=em[:nr, :], op=ALU.mult)
            I_("dve", "tensor_reduce", out=den[:nr, :], in_=gw[:nr, :], axis=AX.X, op=ALU.add)
            I_("dve", "reciprocal", out=den[:nr, :], in_=den[:nr, :])
            I_("dve", "tensor_scalar", out=gw[:nr, :], in0=gw[:nr, :], scalar1=den[:nr, 0:1], scalar2=2.5,
               op0=ALU.mult, op1=ALU.mult)
            if s == 1:
                I_("dve", "tensor_scalar", out=em[:nr, :], in0=em[:nr, :], scalar1=vmask[:nr, 0:1], scalar2=None,
                   op0=ALU.mult)
            I_("dve", "tensor_copy", out=emb[:nr, :], in_=em[:nr, :])
            ps = nps()
            I_("pe", "matmul", out=ps[:nr, :NEXP], lhsT=triu[:nr, :nr], rhs=emb[:nr, :], start=True, stop=True)
            I_("dve", "tensor_tensor", out=pos[:nr, :], in0=ps[:nr, :NEXP], in1=carry[:nr, :], op=ALU.add)
            ps2 = nps()
            I_("pe", "matmul", out=ps2[:, :NEXP], lhsT=L["onesb"][:nr, :], rhs=emb[:nr, :], start=True, stop=True)
            I_("dve", "tensor_tensor", out=carry.v, in0=carry.v, in1=ps2[:, :NEXP], op=ALU.add)
            I_("dve", "tensor_scalar", out=ok[:nr, :], in0=pos[:nr, :], scalar1=float(CAP) - 0.5, scalar2=None, op0=ALU.is_lt)
            I_("dve", "tensor_tensor", out=ok[:nr, :], in0=ok[:nr, :], in1=em[:nr, :], op=ALU.mult)
            I_("dve", "scalar_tensor_tensor", out=key[:nr, :], in0=pos[:nr, :], scalar=1.0, in1=eoff[:nr, :],
               op0=ALU.add, op1=ALU.add)
            I_("dve", "tensor_tensor", out=key[:nr, :], in0=key[:nr, :], in1=ok[:nr, :], op=ALU.mult)
            I_("dve", "tensor_scalar", out=key[:nr, :], in0=key[:nr, :], scalar1=-1.0, scalar2=None, op0=ALU.add)
            I_("dve", "max", out=d8[:nr, :], in_=key[:nr, :])
            I_("dve", "tensor_scalar", out=neg[:nr, :], in0=d8[:nr, :], scalar1=0.0, scalar2=1.0e7, op0=ALU.is_lt, op1=ALU.mult)
            I_("dve", "tensor_tensor", out=neg[:nr, :], in0=neg[:nr, :], in1=d8[:nr, :], op=ALU.add)
            I_("dve", "tensor_copy", out=idx8[:nr, ti, :], in_=neg[:nr, :], disjoint=True)
            for j in range(8):
                I_("dve", "tensor_scalar", out=oh[:nr, :], in0=key[:nr, :], scalar1=d8[:nr, j:j + 1], scalar2=None,
                   op0=ALU.is_equal)
                I_("dve", "tensor_tensor", out=oh[:nr, :], in0=oh[:nr, :], in1=gw[:nr, :], op=ALU.mult)
                I_("dve", "tensor_reduce", out=w8[:nr, ti, j:j + 1], in_=oh[:nr, :], axis=AX.X, op=ALU.add, disjoint=True)
            I_("dve", "tensor_scalar", out=neg[:nr, :], in0=d8[:nr, :], scalar1=0.0, scalar2=None, op0=ALU.is_ge)
            I_("dve", "tensor_tensor", out=w8[:nr, ti, :], in0=w8[:nr, ti, :], in1=neg[:nr, :], op=ALU.mult)
            for j in range(8):
                Dm("pool", indirect=True, out=L["Xs_d"].v, out_offset=(idx8[:nr, ti, j:j + 1], 0), in_=hb[:nr, :],
                   in_offset=None, bounds_check=NEXP * CAP - 1, oob_is_err=False, disjoint=True)


def expert_stage(nc, S, L):
    I_, Dm = S.I, S.D
    nps = L["nps"]
    with S.scope():
        wg = [S.sb("wg%d" % i, [128, KC, 512], BF16) for i in range(2)]
        wu = [S.sb("wu%d" % i, [128, KC, 512], BF16) for i in range(2)]
        wd = [S.sb("wd%d" % i, [128, 4, D], BF16) for i in range(2)]
        Xt = [S.sb("Xt%d" % i, [128, D], BF16) for i in range(2)]
        XTs = [S.sb("XT%d" % i, [128, KC, 512], BF16) for i in range(2)]

        def prep_x(e):
            for sbk in range(CAP // 128):
                x_ = Xt[sbk % 2]
                r0 = e * CAP + sbk * 128
                Dm("sp", out=x_.v, in_=L["Xs_d"].v[r0:r0 + 128, :])
                L["transpose_to"](lambda c: XTs[e % 2][:, c, sbk * 128:(sbk + 1) * 128], x_, 128, KC,
                                  evac=("act" if sbk % 2 else "dve"))
        sl = [S.sb("sl%d" % i, [128, 512], F32) for i in range(2)]
        aT = S.sb("aT", [128, 4, 512], BF16)
        Yt = [S.sb("Yt%d" % i, [128, D], F32) for i in range(2)]
        yi = 0
        for e in range(NEXP + 1):
            g_, u_, d_ = wg[e % 2], wu[e % 2], wd[e % 2]
            Dm("pool", out=g_.v, in_=L["w_eg"].v[e].rearrange("(kc p) n -> p kc n", p=128))
            Dm("pool", out=u_.v, in_=L["w_eu"].v[e].rearrange("(kc p) n -> p kc n", p=128))
            Dm("pool", out=d_.v, in_=L["w_ed"].v[e].rearrange("(kc p) n -> p kc n", p=128))
            tiles = [(0, CAP)] if e < NEXP else TT
            if e == 0:
                prep_x(0)
            for tix, (t0, n) in enumerate(tiles):
                if e < NEXP:
                    XT = XTs[e % 2]
                else:
                    XT = XTs[tix % 2]
                    Dm("sp", out=XT[:, :, :n], in_=L["h2T_d"].v[:, :, t0:t0 + n])
                for hc in range(4):
                    pg, pu = nps(), nps()
                    S.mm(pg[:, :n], [(g_[:, kc, hc * 128:(hc + 1) * 128], XT[:, kc, :n]) for kc in range(KC)])
                    S.mm(pu[:, :n], [(u_[:, kc, hc * 128:(hc + 1) * 128], XT[:, kc, :n]) for kc in range(KC)])
                    s_ = sl[hc % 2]
                    I_("act", "activation", out=s_[:, :n], in_=pg[:, :n], func=AF.Silu)
                    I_("dve", "tensor_tensor", out=aT[:, hc, :n], in0=s_[:, :n], in1=pu[:, :n], op=ALU.mult, disjoint=True)
                if e + 1 < NEXP:
                    prep_x(e + 1)
                for sbk in range((n + 127) // 128):
                    nr = min(128, n - sbk * 128)
                    y_ = Yt[yi % 2]
                    yi += 1
                    for cg in range(4):
                        ps = nps()
                        S.mm(ps[:nr, :], [(aT[:, hc, sbk * 128:sbk * 128 + nr], d_[:, hc, cg * 512:(cg + 1) * 512])
                                          for hc in range(4)])
                        if cg % 2:
                            I_("act", "copy", out=y_[:nr, cg * 512:(cg + 1) * 512], in_=ps[:nr, :], disjoint=True)
                        else:
                            I_("dve", "tensor_copy", out=y_[:nr, cg * 512:(cg + 1) * 512], in_=ps[:nr, :], disjoint=True)
                    if e < NEXP:
                        r0 = e * CAP + t0 + sbk * 128
                        Dm("sp", out=L["Ys_d"].v[r0:r0 + nr, :], in_=y_[:nr, :], disjoint=True)
                    else:
                        r0 = t0 + sbk * 128
                        Dm("sp", out=L["Ysh_d"].v[r0:r0 + nr, :], in_=y_[:nr, :], disjoint=True)


def combine_stage(nc, S, L):
    I_, Dm = S.I, S.D
    idx8, w8 = L["idx8"], L["w8"]
    with S.scope():
        G = [S.sb("G%d" % i, [128, D], F32) for i in range(8)]
        acc = [S.sb("cacc%d" % i, [128, D], F32) for i in range(2)]
        x1t = [S.sb("x1t%d" % i, [128, D], F32) for i in range(2)]
        g2 = [S.sb("g2_%d" % i, [128, D], F32) for i in range(2)]
        gtmp = S.sb("gtmp2", [128, D], F32)
        junk = S.sb("junk2", [128, D], BF16)
        ss = S.sb("ss2", [128, 1], F32)
        rs = S.sb("rs2", [128, 1], F32)
        L["load_bc"](gtmp, L["g_ffn_post"].v[0:1, :], 128)
        for j in range(8):
            I_("pool", "memset", ap=G[j].v.ap, extra_writes=[G[j]], constant=0.0)
        for s in range(2):
            nr = 128 if s == 0 else 64
            L["load_mod"](g2[s], 5, s == 1)
            I_("dve", "tensor_tensor", out=g2[s][:nr, :], in0=g2[s][:nr, :], in1=gtmp[:nr, :], op=ALU.mult)
        for ti, (r0, nr) in enumerate(RT):
            s = 1 if r0 >= T else 0
            a_, x_ = acc[ti % 2], x1t[ti % 2]
            Dm("sp", out=a_[:nr, :], in_=L["Ysh_d"].v[r0:r0 + nr, :])
            Dm("sp", out=x_[:nr, :], in_=L["x1_d"].v[r0:r0 + nr, :])
            for j in range(8):
                Dm("pool", indirect=True, out=G[j][:nr, :], out_offset=None, in_=L["Ys_d"].v,
                   in_offset=(idx8[:nr, ti, j:j + 1], 0), bounds_check=NEXP * CAP - 1, oob_is_err=False)
            for j in range(8):
                I_("dve", "scalar_tensor_tensor", out=a_[:nr, :], in0=G[j][:nr, :], scalar=w8[:nr, ti, j:j + 1],
                   in1=a_[:nr, :], op0=ALU.mult, op1=ALU.add)
            I_("act", "activation", out=junk[:nr, :], in_=a_[:nr, :], func=AF.Square, accum_out=ss[:nr, :])
            L["rstd_from_ss"](rs, ss, nr, D)
            I_("dve", "scalar_tensor_tensor", out=a_[:nr, :], in0=a_[:nr, :], scalar=rs[:nr, 0:1], in1=g2[s][:nr, :],
               op0=ALU.mult, op1=ALU.mult)
            I_("dve", "tensor_tensor", out=a_[:nr, :], in0=a_[:nr, :], in1=x_[:nr, :], op=ALU.add)
            Dm("sp", out=L["y_out"].v[r0:r0 + nr, :], in_=a_[:nr, :], disjoint=True)
```
